# Optimizing a Trainium2 kernel written in Bass

```python
import math
import jax, jax.numpy as jnp
from jax import lax
import numpy as np

D_MODEL = 1024
BATCH = 16
SEQ = 4096
DEPTH = 4

HEAD_DIM = 64
A_WIDTH = D_MODEL // 2
A_HEADS = A_WIDTH // HEAD_DIM
DILATED_BRANCHES = ((128, 1), (512, 4), (2048, 16))
BLOCK = 128
B_WIDTH = D_MODEL // 2
B_GROUPS = 4
B_GROUP_CH = B_WIDTH // B_GROUPS
CHUNK = 128
C_WIDTH = D_MODEL // 2
CONV_WIDTH = 31
D_WIDTH = D_MODEL // 2
D_HEADS = D_WIDTH // HEAD_DIM
N_BUCKETS = 32
MAX_DISTANCE = 2048
N_GROUPS = 4
EXPERTS_PER_GROUP = 4
N_EXPERTS = N_GROUPS * EXPERTS_PER_GROUP
EXPERT_TOP_K = 2
D_EXPERT = D_MODEL // 2
EPS = 1e-6
NEG = -1e30
N_EVEN = (DEPTH + 1) // 2
N_ODD = DEPTH // 2

kernel_name = "hybrid_dilated_gmlp_conformer_stickbreak_hmoe"


def rms_norm(x, g):
    xf = x.astype(jnp.float32)
    y = xf * lax.rsqrt(jnp.mean(xf * xf, -1, keepdims=True) + EPS)
    return (y * g.astype(jnp.float32)).astype(x.dtype)


def layer_norm(x, g, b):
    xf = x.astype(jnp.float32)
    mu = jnp.mean(xf, -1, keepdims=True)
    xc = xf - mu
    var = jnp.mean(xc * xc, -1, keepdims=True)
    y = xc * lax.rsqrt(var + EPS) * g.astype(jnp.float32) + b.astype(jnp.float32)
    return y.astype(x.dtype)


def t5_bucket(dist):
    max_exact = N_BUCKETS // 2
    d = np.maximum(dist, 1).astype(np.float32)
    large = max_exact + (np.log(d / max_exact) / np.log(MAX_DISTANCE / max_exact)
                         * (N_BUCKETS - max_exact)).astype(np.int32)
    large = np.minimum(large, N_BUCKETS - 1)
    return np.where(dist < max_exact, dist, large).astype(np.int32)


def dilated_branch(q, k, v, rel_table, window, dilation):
    b, h, s, hd = q.shape
    n_keys = window // dilation
    sub_len = -(-s // dilation)
    lp = -(-sub_len // BLOCK) * BLOCK
    sp = lp * dilation
    nb = lp // BLOCK
    pad = ((0, 0), (0, 0), (0, sp - s), (0, 0))

    def to_blocks(t):
        t = jnp.pad(t, pad).reshape(b, h, lp, dilation, hd)
        t = jnp.swapaxes(t, 2, 3)
        return t.reshape(b, h, dilation, nb, BLOCK, hd)

    def with_prev(t):
        prev = jnp.pad(t, ((0, 0), (0, 0), (0, 0), (1, 0), (0, 0), (0, 0)))[:, :, :, :-1]
        return jnp.concatenate([prev, t], axis=4)

    qb = to_blocks(q)
    kb = with_prev(to_blocks(k))
    vb = with_prev(to_blocks(v))
    scores = jnp.einsum('bhrnqd,bhrnkd->bhrnqk', qb, kb)

    qi = np.arange(BLOCK)[:, None]
    kj = np.arange(2 * BLOCK)[None, :]
    sub_dist = qi + BLOCK - kj
    in_window = (sub_dist >= 0) & (sub_dist <= n_keys)
    bucket = t5_bucket(np.clip(sub_dist, 0, None) * dilation)
    bias = jnp.transpose(rel_table.astype(jnp.float32)[bucket], (2, 0, 1))
    first_block = (np.arange(nb) == 0)[:, None, None]
    valid = in_window[None] & ~(first_block & (kj < BLOCK)[None])
    logits = jnp.where(valid, scores + bias[None, :, None, None], NEG)

    mx = jnp.max(logits, -1)
    p = jnp.exp(logits - mx[..., None])
    den = jnp.sum(p, -1)
    num = jnp.einsum('bhrnqk,bhrnkd->bhrnqd', p, vb)

    def from_blocks(t):
        rest = t.shape[5:]
        t = t.reshape(b, h, dilation, lp, *rest)
        t = jnp.swapaxes(t, 2, 3).reshape(b, h, sp, *rest)
        return t[:, :, :s]

    return from_blocks(num), from_blocks(den), from_blocks(mx)


def dilated_attention(q, k, v, rel_table):
    b, s, h, hd = q.shape
    qf = jnp.swapaxes(q.astype(jnp.float32), 1, 2) * (hd ** -0.5)
    kf = jnp.swapaxes(k.astype(jnp.float32), 1, 2)
    vf = jnp.swapaxes(v.astype(jnp.float32), 1, 2)
    outs = [dilated_branch(qf, kf, vf, rel_table, w, d) for (w, d) in DILATED_BRANCHES]
    m_all = jnp.max(jnp.stack([m for (_, _, m) in outs]), 0)
    num = sum(n * jnp.exp(m - m_all)[..., None] for (n, _, m) in outs)
    den = sum(l * jnp.exp(m - m_all) for (_, l, m) in outs)
    o = num / den[..., None]
    return jnp.swapaxes(o, 1, 2).reshape(b, s, h * hd)


def spatial_gating(u, v, ln_g, ln_b, w_s, b_s):
    b, s, _ = v.shape
    u = jax.nn.gelu(u)
    v = layer_norm(jax.nn.gelu(v), ln_g, ln_b)
    vc = v.reshape(b, s // CHUNK, CHUNK, B_GROUPS, B_GROUP_CH)
    tril = np.tril(np.ones((CHUNK, CHUNK), dtype=bool))
    w = jnp.where(tril[None], w_s, 0.0)
    mixed = jnp.einsum('gts,bnsgc->bntgc', w, vc) + jnp.swapaxes(b_s, 0, 1)[None, None, :, :, None]
    return u * mixed.reshape(b, s, B_WIDTH).astype(u.dtype)


def conv_module(a, gate, w_dw, b_dw, ln_g, ln_b):
    hcur = a * jax.nn.sigmoid(gate)
    hcur = lax.conv_general_dilated(
        hcur, w_dw[:, None, :].astype(hcur.dtype), window_strides=(1,),
        padding=[(CONV_WIDTH - 1, 0)], dimension_numbers=('NWC', 'WIO', 'NWC'),
        feature_group_count=C_WIDTH) + b_dw
    hcur = layer_norm(hcur, ln_g, ln_b)
    return jax.nn.silu(hcur)


def stick_breaking_attention(q, k, v):
    b, s, h, hd = q.shape
    qf = jnp.transpose(q.astype(jnp.float32), (0, 2, 1, 3)) * (hd ** -0.5)
    kf = jnp.transpose(k.astype(jnp.float32), (0, 2, 1, 3))
    vf = jnp.transpose(v.astype(jnp.float32), (0, 2, 1, 3))
    nb = s // BLOCK
    q_blocks = jnp.moveaxis(qf.reshape(b, h, nb, BLOCK, hd), 2, 0)
    key_pos = jnp.arange(s)

    def block(args):
        qb, i = args
        z = jnp.einsum('bhqd,bhkd->bhqk', qb, kf)
        t = i * BLOCK + jnp.arange(BLOCK)
        causal = key_pos[None, :] < t[:, None]
        log_keep = jnp.where(causal, jax.nn.log_sigmoid(-z), 0.0)
        between = lax.cumsum(log_keep, axis=3, reverse=True) - log_keep
        att = jnp.where(causal, jnp.exp(jax.nn.log_sigmoid(z) + between), 0.0)
        return jnp.einsum('bhqk,bhkd->bhqd', att, vf)

    out = lax.map(block, (q_blocks, jnp.arange(nb)))
    out = jnp.moveaxis(out, 0, 2).reshape(b, h, s, hd)
    return jnp.transpose(out, (0, 2, 1, 3)).reshape(b, s, h * hd)


def hierarchical_moe(hin, w_group, b_group, w_router, b_router, w_gate, w_up, w_down):
    b, s, d = hin.shape
    xt = hin.reshape(b * s, d)
    xf = xt.astype(jnp.float32)
    g_prob = jax.nn.softmax(xf @ w_group.astype(jnp.float32) + b_group.astype(jnp.float32), -1)
    g_w, g_idx = lax.top_k(g_prob, 1)
    e_logits = jnp.einsum('td,gde->tge', xf, w_router.astype(jnp.float32)) + b_router.astype(jnp.float32)
    e_logits = jnp.take_along_axis(e_logits, g_idx[:, :, None], axis=1)[:, 0]
    e_val, e_idx = lax.top_k(e_logits, EXPERT_TOP_K)
    e_w = jax.nn.softmax(e_val, -1)
    flat_idx = g_idx * EXPERTS_PER_GROUP + e_idx
    gates = jnp.sum(jax.nn.one_hot(flat_idx, N_EXPERTS, dtype=jnp.float32)
                    * (g_w * e_w)[..., None], axis=1)
    y = jnp.zeros((b * s, d), jnp.float32)
    for e in range(N_EXPERTS):
        hid = jax.nn.silu(xt @ w_gate[e]) * (xt @ w_up[e])
        y = y + gates[:, e:e + 1] * (hid @ w_down[e]).astype(jnp.float32)
    return y.astype(hin.dtype).reshape(b, s, d)


def setup_inputs(seed: int = 0) -> dict:
    key = jax.random.key(seed)
    ks = iter(jax.random.split(key, 32))

    def nrm(shape, scale):
        return scale * jax.random.normal(next(ks), shape, jnp.float32)

    d = D_MODEL
    ev_in = 3 * A_WIDTH + 2 * B_WIDTH
    od_in = 2 * C_WIDTH + 3 * D_WIDTH
    return {
        "x": nrm((BATCH, SEQ, d), 1.0),
        "c": nrm((BATCH, d), 1.0),
        "rel_bias": nrm((N_BUCKETS, A_HEADS), 0.5),
        "norm1_g": 1.0 + nrm((DEPTH, d), 0.1),
        "norm2_g": 1.0 + nrm((DEPTH, d), 0.1),
        "w_ada": nrm((DEPTH, d, 6 * d), 0.5 * d ** -0.5),
        "b_ada": nrm((DEPTH, 6 * d), 0.02),
        "ev_w_in": nrm((N_EVEN, d, ev_in), d ** -0.5),
        "ev_w_out": nrm((N_EVEN, A_WIDTH + B_WIDTH, d), (A_WIDTH + B_WIDTH) ** -0.5),
        "ev_gmlp_ln_g": 1.0 + nrm((N_EVEN, B_WIDTH), 0.1),
        "ev_gmlp_ln_b": nrm((N_EVEN, B_WIDTH), 0.02),
        "ev_w_s": nrm((N_EVEN, B_GROUPS, CHUNK, CHUNK), CHUNK ** -0.5),
        "ev_b_s": 1.0 + nrm((N_EVEN, B_GROUPS, CHUNK), 0.1),
        "od_w_in": nrm((N_ODD, d, od_in), d ** -0.5),
        "od_w_out": nrm((N_ODD, C_WIDTH + D_WIDTH, d), (C_WIDTH + D_WIDTH) ** -0.5),
        "od_w_dw": nrm((N_ODD, CONV_WIDTH, C_WIDTH), CONV_WIDTH ** -0.5),
        "od_b_dw": nrm((N_ODD, C_WIDTH), 0.02),
        "od_conv_ln_g": 1.0 + nrm((N_ODD, C_WIDTH), 0.1),
        "od_conv_ln_b": nrm((N_ODD, C_WIDTH), 0.02),
        "moe_w_group": nrm((DEPTH, d, N_GROUPS), d ** -0.5),
        "moe_b_group": nrm((DEPTH, N_GROUPS), 0.01),
        "moe_w_router": nrm((DEPTH, N_GROUPS, d, EXPERTS_PER_GROUP), d ** -0.5),
        "moe_b_router": nrm((DEPTH, N_GROUPS, EXPERTS_PER_GROUP), 0.01),
        "moe_w_gate": nrm((DEPTH, N_EXPERTS, d, D_EXPERT), d ** -0.5),
        "moe_w_up": nrm((DEPTH, N_EXPERTS, d, D_EXPERT), d ** -0.5),
        "moe_w_down": nrm((DEPTH, N_EXPERTS, D_EXPERT, d), D_EXPERT ** -0.5),
        "final_norm_g": 1.0 + nrm((d,), 0.1),
    }


def reference(x, c, rel_bias, norm1_g, norm2_g, w_ada, b_ada, ev_w_in, ev_w_out,
              ev_gmlp_ln_g, ev_gmlp_ln_b, ev_w_s, ev_b_s, od_w_in, od_w_out, od_w_dw,
              od_b_dw, od_conv_ln_g, od_conv_ln_b, moe_w_group, moe_b_group, moe_w_router,
              moe_b_router, moe_w_gate, moe_w_up, moe_w_down, final_norm_g):
    b, s, _ = x.shape
    for layer in range(DEPTH):
        j = layer // 2
        mod = jax.nn.silu(c) @ w_ada[layer] + b_ada[layer]
        sh1, sc1, g1, sh2, sc2, g2 = jnp.split(mod[:, None, :], 6, axis=-1)
        hcur = rms_norm(x, norm1_g[layer]) * (1 + sc1) + sh1
        if layer % 2 == 0:
            proj = hcur @ ev_w_in[j]
            q, k, v, u, gv = jnp.split(
                proj, [A_WIDTH, 2 * A_WIDTH, 3 * A_WIDTH, 3 * A_WIDTH + B_WIDTH], axis=-1)
            o_a = dilated_attention(q.reshape(b, s, A_HEADS, HEAD_DIM),
                                    k.reshape(b, s, A_HEADS, HEAD_DIM),
                                    v.reshape(b, s, A_HEADS, HEAD_DIM), rel_bias)
            o_b = spatial_gating(u, gv, ev_gmlp_ln_g[j], ev_gmlp_ln_b[j], ev_w_s[j], ev_b_s[j])
            mix = jnp.concatenate([o_a.astype(x.dtype), o_b.astype(x.dtype)], -1) @ ev_w_out[j]
        else:
            proj = hcur @ od_w_in[j]
            a, gate, q, k, v = jnp.split(
                proj, [C_WIDTH, 2 * C_WIDTH, 2 * C_WIDTH + D_WIDTH, 2 * C_WIDTH + 2 * D_WIDTH], axis=-1)
            o_c = conv_module(a, gate, od_w_dw[j], od_b_dw[j], od_conv_ln_g[j], od_conv_ln_b[j])
            o_d = stick_breaking_attention(q.reshape(b, s, D_HEADS, HEAD_DIM),
                                           k.reshape(b, s, D_HEADS, HEAD_DIM),
                                           v.reshape(b, s, D_HEADS, HEAD_DIM))
            mix = jnp.concatenate([o_c.astype(x.dtype), o_d.astype(x.dtype)], -1) @ od_w_out[j]
        x = x + g1 * mix
        hcur = rms_norm(x, norm2_g[layer]) * (1 + sc2) + sh2
        x = x + g2 * hierarchical_moe(hcur, moe_w_group[layer], moe_b_group[layer],
                                      moe_w_router[layer], moe_b_router[layer],
                                      moe_w_gate[layer], moe_w_up[layer], moe_w_down[layer])
    return rms_norm(x, final_norm_g)
```

```python
import numpy as np
import concourse.bass as bass
import concourse.mybir as mybir
from concourse.bass_utils import run_bass_kernel_spmd
from contextlib import ExitStack

F32 = mybir.dt.float32
BF16 = mybir.dt.bfloat16
AF = mybir.ActivationFunctionType
ALU = mybir.AluOpType
AX = mybir.AxisListType

D = 1024
S = 4096
DEPTH = 4
NCORES = 8
EPS = 1e-6
NEG = -1e30
BRANCHES = ((128, 1), (512, 4), (2048, 16))

ENGS = ("pe", "act", "dve", "pool", "sp")
DMA_K = 12


class Buf:
    __slots__ = ("name", "lw", "rd", "lws")

    def __init__(self, name=""):
        self.name = name
        self.lw = None
        self.rd = []
        self.lws = []


class Op:
    __slots__ = ("eng", "fn", "deps", "pos", "tick", "dma", "dj", "need")


class Prog:
    def __init__(self, nc):
        self.nc = nc
        self.ops = {e: [] for e in ENGS}
        self.ndma = {e: 0 for e in ENGS}
        self.pending_dma = []

    def add(self, eng, fn, reads=(), writes=(), dma=False, extra_deps=None):
        op = Op()
        op.eng = eng
        op.fn = fn
        op.dma = dma
        op.pos = len(self.ops[eng])
        op.tick = None
        op.need = False
        op.dj = None
        deps = set()
        for b in reads:
            for w_ in b.lws:
                deps.add(w_)
        for b in writes:
            for w_ in b.lws:
                deps.add(w_)
            for r in b.rd:
                deps.add(r)
        if extra_deps:
            deps |= set(extra_deps)
        deps.discard(op)
        op.deps = deps
        if dma:
            op.dj = self.ndma[eng]
            self.ndma[eng] += 1
            self.pending_dma.append(op)
        wset = set(id(b) for b in writes)
        for b in writes:
            if dma and b.lws and all(w_.dma for w_ in b.lws):
                b.lws = b.lws[-24:] + [op]
            else:
                b.lws = [op]
            b.lw = op
            b.rd = []
        for b in reads:
            if id(b) in wset:
                continue
            if not dma:
                b.rd = [r for r in b.rd if r.dma or r.eng != eng]
            b.rd.append(op)
        self.ops[eng].append(op)
        return op

    def barrier(self):
        deps = set(self.pending_dma)
        for e in ENGS:
            for o in reversed(self.ops[e]):
                if o.fn is not None and not o.dma:
                    deps.add(o)
                    break
        for e in ENGS:
            self.add(e, None, extra_deps=deps)
        self.pending_dma = []

    def dma(self, q, out, in_, reads=(), writes=(), **kw):
        return self.add(q, lambda e: e.dma_start(out=out, in_=in_, **kw), reads, writes, dma=True)

    def mm(self, out, lhsT, rhs, start, stop, reads=(), writes=(), **kw):
        return self.add("pe", lambda e: e.matmul(out, lhsT, rhs, start=start, stop=stop, **kw),
                        reads, writes)

    def tr(self, out, in_, ident, reads=(), writes=()):
        return self.add("pe", lambda e: e.transpose(out, in_, ident), reads, writes)

    def act(self, out, in_, func, reads=(), writes=(), **kw):
        return self.add("act", lambda e: e.activation(out, in_, func, **kw), reads, writes)

    def _hazard(self, op, d):
        if d.dma or op.dma:
            return True
        if op.eng == "pe":
            return False
        return d.pos >= op.pos - 2

    def emit(self, stack):
        nc = self.nc
        sem = {e: stack.enter_context(nc.semaphore("s_" + e)) for e in ENGS}
        dsem = {e: [stack.enter_context(nc.semaphore("d_%s_%d" % (e, i))) for i in range(DMA_K)]
                for e in ENGS if self.ndma[e] > 0}
        for e in ENGS:
            for op in self.ops[e]:
                for d in op.deps:
                    if d.dma:
                        continue
                    if d.eng != e or self._hazard(op, d):
                        d.need = True
        for e in ENGS:
            t = 0
            for op in self.ops[e]:
                if op.dma or op.fn is None:
                    continue
                if op.need:
                    t += 1
                    op.tick = t
        self.nticks = {e: sum(1 for o in self.ops[e] if o.tick) for e in ENGS}
        block = stack.enter_context(nc.Block())
        nwaits = {e: 0 for e in ENGS}

        def run(e, eng):
            seen = {x: 0 for x in ENGS}
            dseen = {}
            for op in self.ops[e]:
                waits = {}
                dwaits = {}
                for d in op.deps:
                    if d.dma:
                        key = (d.eng, d.dj % DMA_K)
                        val = 16 * (d.dj // DMA_K + 1)
                        if dseen.get(key, 0) < val:
                            dwaits[key] = max(dwaits.get(key, 0), val)
                    else:
                        if d.fn is None:
                            continue
                        if d.eng == e and not self._hazard(op, d):
                            continue
                        if seen[d.eng] < d.tick:
                            waits[d.eng] = max(waits.get(d.eng, 0), d.tick)
                if op.dma and op.dj >= DMA_K:
                    key = (e, op.dj % DMA_K)
                    val = 16 * (op.dj // DMA_K)
                    if dseen.get(key, 0) < val:
                        dwaits[key] = max(dwaits.get(key, 0), val)
                for x, v in waits.items():
                    eng.wait_ge(sem[x], v)
                    seen[x] = v
                    nwaits[e] += 1
                for key, v in dwaits.items():
                    eng.wait_ge(dsem[key[0]][key[1]], v)
                    dseen[key] = v
                    nwaits[e] += 1
                if op.fn is None:
                    continue
                ins = op.fn(eng)
                if op.dma:
                    ins.then_inc(dsem[e][op.dj % DMA_K], 16)
                elif op.tick is not None:
                    ins.then_inc(sem[e], 1)
            if self.ndma[e] > 0:
                n = self.ndma[e]
                for i in range(min(DMA_K, n)):
                    cnt = (n - 1 - i) // DMA_K + 1
                    eng.wait_ge(dsem[e][i], 16 * cnt)

        @block.tensor
        def _(eng):
            run("pe", eng)

        @block.scalar
        def _(eng):
            run("act", eng)

        @block.vector
        def _(eng):
            run("dve", eng)

        @block.gpsimd
        def _(eng):
            run("pool", eng)

        @block.sync
        def _(eng):
            run("sp", eng)

        self.nwaits = nwaits


class Arena:
    def __init__(self, nc, st, nbytes):
        self.t16 = st.enter_context(nc.sbuf_tensor("arena", [128, nbytes // 2], BF16))
        self.t32 = self.t16.bitcast(F32)
        self.nbytes = nbytes
        self.off = 0
        self.floor = 0

    def reset(self):
        self.off = self.floor

    def alloc(self, free, dt, name=""):
        if isinstance(free, int):
            free = (free,)
        n = 1
        for f in free:
            n *= f
        es = 2 if dt == BF16 else 4
        off = (self.off + 63) // 64 * 64
        self.off = off + n * es
        assert self.off <= self.nbytes, ("SBUF arena overflow", name, self.off)
        base = self.t16 if dt == BF16 else self.t32
        eo = off // es
        ap = base[:, eo:eo + n]
        if len(free) == 2:
            ap = ap.rearrange("p (a b) -> p a b", b=free[1])
        elif len(free) == 3:
            ap = ap.rearrange("p (a b c) -> p a b c", b=free[1], c=free[2])
        elif len(free) == 4:
            ap = ap.rearrange("p (a b c d) -> p a b c d", b=free[1], c=free[2], d=free[3])
        return ap, Buf(name)


def _t5_bucket(dist):
    n_buckets, max_distance = 32, 2048
    max_exact = n_buckets // 2
    d = np.maximum(dist, 1).astype(np.float32)
    large = max_exact + (np.log(d / max_exact) / np.log(max_distance / max_exact)
                         * (n_buckets - max_exact)).astype(np.int32)
    large = np.minimum(large, n_buckets - 1)
    return np.where(dist < max_exact, dist, large).astype(np.int32)


def _bias_tables(rel_bias):
    out = np.full((4, 128, 3, 2, 2, 128), NEG, np.float32)
    k = np.arange(128)[:, None]
    q = np.arange(128)[None, :]
    for bi, (w, d) in enumerate(BRANCHES):
        for kb in range(2):
            sub = q + 128 - (k + 128 * kb)
            valid = (sub >= 0) & (sub <= w // d)
            bucket = _t5_bucket(np.clip(sub, 0, None) * d)
            for hp in range(4):
                for h in range(2):
                    g = rel_bias[bucket, hp * 2 + h]
                    out[hp, :, bi, h, kb, :] = np.where(valid, g, np.float32(NEG))
    return out


def _consts():
    c = {}
    c["ident"] = np.eye(128, dtype=np.float32)
    k = np.arange(128)[:, None]
    q = np.arange(512)[None, :]
    sb = np.stack([((i * 128 + k) < q).astype(np.float32) for i in range(4)], 1)
    c["sbmask"] = sb
    c["utri"] = (np.arange(128)[:, None] >= np.arange(128)[None, :]).astype(np.float32)
    c["tril_st"] = (np.arange(128)[:, None] <= np.arange(128)[None, :]).astype(np.float32)
    return c


class Ctx:
    pass


def build(nseq, stop_after=None, debug=False):
    nc = bass.Bass("TRN2", target_bir_lowering=False)
    T = nseq * S
    ctx = Ctx()
    ctx.nc = nc
    ctx.nseq = nseq
    ctx.debug = debug
    d = {}

    def din(name, shape, dt=F32):
        d[name] = nc.dram_tensor(name, list(shape), dt, kind="ExternalInput").ap()

    def dscr(name, shape, dt):
        kind = "ExternalOutput" if debug else "Internal"
        d[name] = nc.dram_tensor(name, list(shape), dt, kind=kind).ap()

    din("x", (T, D))
    din("c", (nseq, D))
    din("biasT", (4, 128, 3, 2, 2, 128))
    din("norm1_g", (DEPTH, D))
    din("norm2_g", (DEPTH, D))
    din("w_ada", (DEPTH, D, 6 * D))
    din("b_ada", (DEPTH, 6 * D))
    din("ev_w_in", (2, D, 2560))
    din("ev_w_out", (2, D, D))
    din("ev_gmlp_ln_g", (2, 512))
    din("ev_gmlp_ln_b", (2, 512))
    din("ev_w_sT", (2, 4, 128, 128))
    din("ev_b_s", (2, 4, 128))
    din("od_w_in", (2, D, 2560))
    din("od_w_out", (2, D, D))
    din("od_w_dw", (2, 31, 512))
    din("od_b_dw", (2, 512))
    din("od_conv_ln_g", (2, 512))
    din("od_conv_ln_b", (2, 512))
    din("moe_wr", (DEPTH, D, 20))
    din("moe_br", (DEPTH, 20))
    din("moe_w_gate", (DEPTH, 16, D, 512))
    din("moe_w_up", (DEPTH, 16, D, 512))
    din("moe_w_down", (DEPTH, 16, 512, D))
    din("final_norm_g", (D,))
    din("ident", (128, 128))
    din("sbmask", (128, 4, 512))
    din("utri", (128, 128))
    din("tril_st", (128, 128))
    d["out"] = nc.dram_tensor("out", [T, D], F32, kind="ExternalOutput").ap()
    dscr("xs", (T, D), F32)
    dscr("mod", (DEPTH, nseq, 6 * D), F32)
    dscr("qT", (nseq, 4, 128, S), BF16)
    dscr("kT", (nseq, 4, 128, S), BF16)
    dscr("vv", (nseq, S, 512), BF16)
    dscr("mixT", (nseq, 8, 128, S), BF16)
    dscr("hcT", (nseq, 4, 128, S), F32)
    dscr("h2T", (nseq, 8, 128, S), BF16)
    if debug:
        dscr("gates_dbg", (T, 16), F32)
        dscr("yacc_dbg", (T, D), F32)
        dscr("conv_dbg", (5, 128, 512), F32)
    ctx.d = d
    ctx.R = {}

    def R(name, *key):
        k = (name,) + key
        if k not in ctx.R:
            ctx.R[k] = Buf(str(k))
        return ctx.R[k]
    ctx.Rf = R

    P = Prog(nc)
    ctx.P = P
    with ExitStack() as st:
        A = Arena(nc, st, 206 * 1024)
        ctx.A = A
        ctx.PS = []
        ctx.PSB = []
        for i in range(8):
            ctx.PS.append(st.enter_context(nc.psum_tensor("ps%d" % i, [128, 512], F32))[:, :])
            ctx.PSB.append(Buf("ps%d" % i))
        ctx.ident, ctx.identB = A.alloc(128, F32, "ident")
        P.dma("sp", ctx.ident, d["ident"], writes=[ctx.identB])
        ctx.gates, ctx.gatesB = A.alloc((nseq * 32, 16), F32, "gates")
        A.floor = A.off

        phases = [("mods", None)]
        for l in range(DEPTH):
            phases += [("A", l), ("B", l), ("C", l), ("D", l)]
        for ph in phases:
            if ph[0] == "mods":
                phase_mods(ctx)
            elif ph[0] == "A":
                phase_A(ctx, ph[1])
            elif ph[0] == "B":
                if ph[1] % 2 == 0:
                    phase_B_even(ctx, ph[1])
                else:
                    phase_B_odd(ctx, ph[1])
            elif ph[0] == "C":
                phase_C(ctx, ph[1])
            elif ph[0] == "D":
                phase_D(ctx, ph[1])
            if stop_after is not None and ph == stop_after:
                break
        P.emit(st)
    ctx.prog = P
    return nc, ctx


def phase_mods(ctx):
    P, A, d, nseq = ctx.P, ctx.A, ctx.d, ctx.nseq
    P.barrier()
    A.reset()
    cT, cTB = A.alloc((8, nseq), F32, "cT")
    sc, scB = A.alloc((8, nseq), F32, "sc")
    for s_ in range(nseq):
        P.dma("sp", cT[:, :, s_], d["c"][s_].rearrange("(c p) -> p c", p=128), writes=[cTB],
              allow_slow_non_contiguous=True)
    P.act(sc, cT, AF.Silu, reads=[cTB], writes=[scB])
    bada, badaB = A.alloc(6 * D, F32, "bada")
    modsb, modsbB = A.alloc(6 * D, F32, "modsb")
    wb = [A.alloc((8, 512), F32, "wada%d" % i) for i in range(3)]
    i = 0
    for l in range(DEPTH):
        for s in range(nseq):
            P.dma("sp", bada[s:s + 1], d["b_ada"][l:l + 1, :], writes=[badaB])
        for cb in range(12):
            w, wB = wb[i % 3]
            P.dma("sp", w, d["w_ada"][l].rearrange("(c p) n -> p c n", p=128)[:, :, cb * 512:(cb + 1) * 512],
                  writes=[wB])
            ps, psB = ctx.PS[i % 2], ctx.PSB[i % 2]
            for c in range(8):
                P.mm(ps[0:nseq, :], sc[:, c, :], w[:, c, :], c == 0, c == 7, reads=[scB, wB], writes=[psB])
            P.add("dve", lambda e, ps=ps, cb=cb: e.tensor_tensor(
                modsb[0:nseq, cb * 512:(cb + 1) * 512], ps[0:nseq, :],
                bada[0:nseq, cb * 512:(cb + 1) * 512], ALU.add),
                reads=[psB, badaB], writes=[modsbB])
            i += 1
        P.dma("sp", d["mod"][l], modsb[0:nseq], reads=[modsbB], writes=[ctx.Rf("mod")])


def load_mod_fm(ctx, layer, s, which, A, name):
    P, d = ctx.P, ctx.d
    t, tB = A.alloc(8, F32, name)
    src = d["mod"][layer, s, which * D:(which + 1) * D].rearrange("(c p) -> p c", p=128)
    P.dma("sp", t, src, reads=[ctx.Rf("mod")], writes=[tB], allow_slow_non_contiguous=True)
    return t, tB


def load_vec_fm(ctx, vec_ap, A, name, n=8):
    P = ctx.P
    t, tB = A.alloc(n, F32, name)
    P.dma("sp", t, vec_ap.rearrange("(c p) -> p c", p=128), writes=[tB], allow_slow_non_contiguous=True)
    return t, tB


def load_bcast(ctx, vec_ap, n, A, name, reads=()):
    P = ctx.P
    t, tB = A.alloc(n, F32, name)
    src = bass.AP(vec_ap.tensor, vec_ap.offset, [[0, 128], [1, n]])
    P.dma("sp", t, src, reads=list(reads), writes=[tB])
    return t, tB


def make_gs_sh(ctx, layer, s, which_sh, which_sc, gvec_ap, A, tag):
    P = ctx.P
    sh, shB = load_mod_fm(ctx, layer, s, which_sh, A, "sh" + tag)
    scv, scB = load_mod_fm(ctx, layer, s, which_sc, A, "sc" + tag)
    g, gB = load_vec_fm(ctx, gvec_ap, A, "g" + tag)
    gs, gsB = A.alloc(8, F32, "gs" + tag)
    P.add("dve", lambda e: e.scalar_tensor_tensor(gs, scv, 1.0, g, ALU.add, ALU.mult),
          reads=[scB, gB], writes=[gsB])
    return gs, gsB, sh, shB


def nmt(ctx, xt, xtB, gs, gsB, sh, shB, hT, hTB, scr, h32=None, router=None):
    P = ctx.P
    ss, ssB = scr["ss"]
    lnv, lnvB = scr["lnv"]
    rstd, rstdB = scr["rstd"]
    junk, junkB = scr["junk"]
    P.add("pool", lambda e: e.memset(ss, 0.0), writes=[ssB])
    for t in range(4):
        P.act(junk, xt[:, t, :], AF.Square, reads=[xtB], writes=[junkB, ssB], accum_out=ss[:, t:t + 1])
    P.act(lnv, ss, AF.Ln, reads=[ssB], writes=[lnvB], scale=1.0 / D, bias=EPS)
    P.act(rstd, lnv, AF.Exp, reads=[lnvB], writes=[rstdB], scale=-0.5)
    for t in range(4):
        P.add("dve", lambda e, t=t: e.tensor_scalar(xt[:, t, :], xt[:, t, :], rstd[:, t:t + 1], None, op0=ALU.mult),
              reads=[xtB, rstdB], writes=[xtB])
    for c in range(8):
        ps, psB = ctx.PS[c % 2], ctx.PSB[c % 2]
        for t in range(4):
            P.tr(ps[:, t * 128:(t + 1) * 128], xt[:, t, c * 128:(c + 1) * 128], ctx.ident,
                 reads=[xtB, ctx.identB], writes=[psB])
        if h32 is None:
            P.act(hT[:, c, :], ps, AF.Identity, reads=[psB, gsB, shB], writes=[hTB],
                  scale=gs[:, c:c + 1], bias=sh[:, c:c + 1])
        else:
            h, hB = h32[c % 2]
            P.act(h, ps, AF.Identity, reads=[psB, gsB, shB], writes=[hB],
                  scale=gs[:, c:c + 1], bias=sh[:, c:c + 1])
            P.add("dve", lambda e, h=h, c=c: e.tensor_copy(hT[:, c, :], h), reads=[hB], writes=[hTB])
            router(c, h, hB)


def phase_A(ctx, layer):
    P, A, d, nseq = ctx.P, ctx.A, ctx.d, ctx.nseq
    even = layer % 2 == 0
    j = layer // 2
    P.barrier()
    A.reset()
    PS, PSB = ctx.PS, ctx.PSB
    win, winB = A.alloc((8, 2560), BF16, "win")
    P.dma("pool", win, d["ev_w_in" if even else "od_w_in"][j].rearrange("(c p) n -> p c n", p=128), writes=[winB])
    scr = {k: A.alloc(4, F32, k) for k in ("ss", "lnv", "rstd")}
    scr["junk"] = A.alloc(1024, BF16, "junk")
    xb = [A.alloc((4, 1024), F32, "xt%d" % i) for i in range(2)]
    hTb = [A.alloc((8, 512), BF16, "hT%d" % i) for i in range(2)]
    qst = [A.alloc((4, 512), BF16, "qst%d" % i) for i in range(2)]
    kst = [A.alloc((4, 512), BF16, "kst%d" % i) for i in range(2)]
    vst = [A.alloc((4, 512), BF16, "vst%d" % i) for i in range(2)]
    if even:
        wsf, wsfB = A.alloc((4, 128), F32, "wsf")
        P.dma("sp", wsf, d["ev_w_sT"][j].rearrange("g s t -> s g t"), writes=[wsfB])
        tril, trilB = A.alloc(128, F32, "tril")
        P.dma("sp", tril, d["tril_st"], writes=[trilB])
        wsm, wsmB = A.alloc((4, 128), BF16, "wsm")
        for g in range(4):
            P.add("dve", lambda e, g=g: e.tensor_tensor(wsm[:, g, :], wsf[:, g, :], tril, ALU.mult),
                  reads=[wsfB, trilB], writes=[wsmB])
        bsf, bsfB = A.alloc(512, F32, "bsf")
        P.dma("sp", bsf[0:1], d["ev_b_s"][j].rearrange("(o g) t -> o (g t)", o=1), writes=[bsfB])
        bsr, bsrB = A.alloc(512, BF16, "bsr")
        onesr, onesrB = A.alloc(128, BF16, "onesr")
        P.add("dve", lambda e: e.tensor_copy(bsr[0:1], bsf[0:1]), reads=[bsfB], writes=[bsrB])
        P.add("dve", lambda e: e.memset(onesr[0:1], 1.0), writes=[onesrB])
        lng, lngB = load_bcast(ctx, d["ev_gmlp_ln_g"][j], 512, A, "lng")
        lnb, lnbB = load_bcast(ctx, d["ev_gmlp_ln_b"][j], 512, A, "lnb")
        ug = [A.alloc((4, 512), BF16, "ug%d" % i) for i in range(2)]
        gg = [A.alloc((4, 512), F32, "gg%d" % i) for i in range(2)]
        vg = [A.alloc((4, 512), BF16, "vg%d" % i) for i in range(2)]
        obst = [A.alloc((4, 512), BF16, "obst%d" % i) for i in range(2)]
        st1 = [A.alloc(4, F32, "s1_%d" % i) for i in range(2)]
        st2 = [A.alloc(4, F32, "s2_%d" % i) for i in range(2)]
        mean = A.alloc(4, F32, "mean")
        msq = A.alloc(4, F32, "msq")
        var = A.alloc(4, F32, "var")
        rs2 = A.alloc(4, F32, "rs2")
    else:
        hcst = [A.alloc((4, 512), F32, "hcst%d" % i) for i in range(2)]
        sg = [A.alloc(512, F32, "sg%d" % i) for i in range(2)]
    src_x = d["x"] if layer == 0 else d["xs"]
    bi = 0
    for s in range(nseq):
        gs, gsB, sh, shB = make_gs_sh(ctx, layer, s, 0, 1, d["norm1_g"][layer], A, "1_%d" % s)
        for blk in range(8):
            t0 = s * S + blk * 512
            cols = slice(blk * 512, (blk + 1) * 512)
            xt, xtB = xb[bi % 2]
            hT, hTB = hTb[bi % 2]
            rd = [] if layer == 0 else [ctx.Rf("xs", s, blk)]
            P.dma("sp", xt, src_x[t0:t0 + 512, :].rearrange("(t p) dd -> p t dd", p=128), reads=rd, writes=[xtB])
            nmt(ctx, xt, xtB, gs, gsB, sh, shB, hT, hTB, scr)
            qs, qsB = qst[bi % 2]
            ks, ksB = kst[bi % 2]
            vs, vsB = vst[bi % 2]
            if even:
                qcol, kcol, vcol = 0, 512, 1024
            else:
                qcol, kcol, vcol = 1024, 1536, 2048
            n_fm = 0
            for oc in range(8):
                col0 = (qcol if oc < 4 else kcol) + (oc % 4) * 128
                ps, psB = PS[2 + n_fm % 2], PSB[2 + n_fm % 2]
                n_fm += 1
                for c in range(8):
                    P.mm(ps, win[:, c, col0:col0 + 128], hT[:, c, :], c == 0, c == 7, reads=[winB, hTB], writes=[psB])
                if oc < 4:
                    P.act(qs[:, oc, :], ps, AF.Identity, reads=[psB], writes=[qsB], scale=0.125)
                else:
                    P.add("dve", lambda e, ps=ps, oc=oc, ks=ks: e.tensor_copy(ks[:, oc - 4, :], ps), reads=[psB], writes=[ksB])
            P.dma("sp", d["qT"][s].rearrange("h p t -> p h t")[:, :, cols], qs, reads=[qsB], writes=[ctx.Rf("qT", s)])
            P.dma("sp", d["kT"][s].rearrange("h p t -> p h t")[:, :, cols], ks, reads=[ksB], writes=[ctx.Rf("kT", s)])
            if even:
                u, uB = ug[bi % 2]
                for oc in range(4):
                    col0 = 1536 + oc * 128
                    ps, psB = PS[2 + n_fm % 2], PSB[2 + n_fm % 2]
                    n_fm += 1
                    for c in range(8):
                        P.mm(ps, win[:, c, col0:col0 + 128], hT[:, c, :], c == 0, c == 7, reads=[winB, hTB], writes=[psB])
                    P.act(u[:, oc, :], ps, AF.Gelu_apprx_tanh, reads=[psB], writes=[uB])
            else:
                hc, hcB = hcst[bi % 2]
                for oc in range(4):
                    psa, psaB = PS[2], PSB[2]
                    psg, psgB = PS[3], PSB[3]
                    for c in range(8):
                        P.mm(psa, win[:, c, oc * 128:(oc + 1) * 128], hT[:, c, :], c == 0, c == 7, reads=[winB, hTB], writes=[psaB])
                    for c in range(8):
                        P.mm(psg, win[:, c, 512 + oc * 128:512 + (oc + 1) * 128], hT[:, c, :], c == 0, c == 7, reads=[winB, hTB], writes=[psgB])
                    sgt, sgtB = sg[oc % 2]
                    P.act(sgt, psg, AF.Sigmoid, reads=[psgB], writes=[sgtB])
                    P.add("dve", lambda e, psa=psa, sgt=sgt, hc=hc, oc=oc: e.tensor_tensor(hc[:, oc, :], psa, sgt, ALU.mult),
                          reads=[psaB, sgtB], writes=[hcB])
                P.dma("sp", d["hcT"][s].rearrange("h p t -> p h t")[:, :, cols], hc, reads=[hcB], writes=[ctx.Rf("hcT", s)])
            if even:
                s1, s1B = st1[bi % 2]
                s2, s2B = st2[bi % 2]
                vgt, vgB = vg[bi % 2]
                g4, g4B = gg[bi % 2]
                P.add("pool", lambda e, s1=s1: e.memset(s1, 0.0), writes=[s1B])
                P.add("pool", lambda e, s2=s2: e.memset(s2, 0.0), writes=[s2B])
            for t in range(4):
                ps, psB = PS[4 + t % 2], PSB[4 + t % 2]
                for c in range(8):
                    P.mm(ps, hT[:, c, t * 128:(t + 1) * 128], win[:, c, vcol:vcol + 512], c == 0, c == 7, reads=[winB, hTB], writes=[psB])
                P.add("dve", lambda e, ps=ps, t=t, vs=vs: e.tensor_copy(vs[:, t, :], ps), reads=[psB], writes=[vsB])
                if even:
                    ps2, ps2B = PS[6 + t % 2], PSB[6 + t % 2]
                    for c in range(8):
                        P.mm(ps2, hT[:, c, t * 128:(t + 1) * 128], win[:, c, 2048:2560], c == 0, c == 7, reads=[winB, hTB], writes=[ps2B])
                    P.act(g4[:, t, :], ps2, AF.Gelu_apprx_tanh, reads=[ps2B], writes=[g4B, s1B], accum_out=s1[:, t:t + 1])
                    P.act(scr["junk"][0][:, 0:512], g4[:, t, :], AF.Square, reads=[g4B], writes=[scr["junk"][1], s2B], accum_out=s2[:, t:t + 1])
            P.dma("sp", d["vv"][s, blk * 512:(blk + 1) * 512, :].rearrange("(t p) n -> p t n", p=128), vs, reads=[vsB], writes=[ctx.Rf("vv", s)])
            if even:
                mn, mnB = mean
                mq, mqB = msq
                vr, vrB = var
                r2, r2B = rs2
                P.add("dve", lambda e, s1=s1, mn=mn: e.tensor_scalar(mn, s1, 1.0 / 512, None, op0=ALU.mult), reads=[s1B], writes=[mnB])
                P.add("dve", lambda e, mn=mn, mq=mq: e.tensor_tensor(mq, mn, mn, ALU.mult), reads=[mnB], writes=[mqB])
                P.add("dve", lambda e, s2=s2, mq=mq, vr=vr: e.scalar_tensor_tensor(vr, s2, 1.0 / 512, mq, ALU.mult, ALU.subtract), reads=[s2B, mqB], writes=[vrB])
                P.act(vr, vr, AF.Ln, reads=[vrB], writes=[vrB], bias=EPS)
                P.act(r2, vr, AF.Exp, reads=[vrB], writes=[r2B], scale=-0.5)
                for t in range(4):
                    P.add("dve", lambda e, t=t, g4=g4, mn=mn, r2=r2: e.tensor_scalar(
                        g4[:, t, :], g4[:, t, :], mn[:, t:t + 1], r2[:, t:t + 1], op0=ALU.subtract, op1=ALU.mult),
                        reads=[g4B, mnB, r2B], writes=[g4B])
                    P.add("pool", lambda e, t=t, g4=g4: e.tensor_tensor(g4[:, t, :], g4[:, t, :], lng, ALU.mult),
                          reads=[g4B, lngB], writes=[g4B])
                    P.add("dve", lambda e, t=t, g4=g4, vgt=vgt: e.tensor_tensor(vgt[:, t, :], g4[:, t, :], lnb, ALU.add),
                          reads=[g4B, lnbB], writes=[vgB])
                ob, obB = obst[bi % 2]
                for g in range(4):
                    ps, psB = PS[2 + g % 2], PSB[2 + g % 2]
                    for t in range(4):
                        P.mm(ps[:, t * 128:(t + 1) * 128], vgt[:, t, g * 128:(g + 1) * 128], wsm[:, g, :], True, False,
                             reads=[vgB, wsmB], writes=[psB])
                        P.mm(ps[:, t * 128:(t + 1) * 128], onesr[0:1, :], bsr[0:1, g * 128:(g + 1) * 128], False, True,
                             reads=[onesrB, bsrB], writes=[psB])
                    P.add("dve", lambda e, ps=ps, g=g, ob=ob, u=u: e.tensor_tensor(ob[:, g, :], ps, u[:, g, :], ALU.mult),
                          reads=[psB, uB], writes=[obB])
                P.dma("sp", d["mixT"][s, 4:8].rearrange("h p t -> p h t")[:, :, cols], ob, reads=[obB], writes=[ctx.Rf("mixT", s, 1)])
            bi += 1


def phase_B_even(ctx, layer):
    P, A, d, nseq = ctx.P, ctx.A, ctx.d, ctx.nseq
    PS, PSB = ctx.PS, ctx.PSB
    P.barrier()
    A.reset()
    qh, qhB = A.alloc(S, BF16, "qh")
    kh, khB = A.alloc(S, BF16, "kh")
    vraw = [A.alloc((32, 128), BF16, "vraw%d" % b) for b in range(3)]
    va0 = [A.alloc((32, 128), BF16, "va0_%d" % b) for b in range(3)]
    v0b = [A.alloc((32, 128), BF16, "v0b_%d" % b) for b in range(3)]
    onesA0, onesA0B = A.alloc(128, BF16, "onesA0")
    ones0B, ones0BB = A.alloc(128, BF16, "ones0B")
    P.add("pool", lambda e: e.memset(onesA0[:, 0:64], 1.0), writes=[onesA0B])
    P.add("pool", lambda e: e.memset(onesA0[:, 64:128], 0.0), writes=[onesA0B])
    P.add("pool", lambda e: e.memset(ones0B[:, 0:64], 0.0), writes=[ones0BB])
    P.add("pool", lambda e: e.memset(ones0B[:, 64:128], 1.0), writes=[ones0BB])
    for b in range(3):
        P.add("pool", lambda e, b=b: e.memset(va0[b][0][:, :, 64:128], 0.0), writes=[va0[b][1]])
        P.add("pool", lambda e, b=b: e.memset(v0b[b][0][:, :, 0:64], 0.0), writes=[v0b[b][1]])
    bT, bTB = A.alloc((3, 2, 2, 128), F32, "biasT")
    acc, accB = A.alloc((2, S), F32, "acc")
    obf, obfB = A.alloc(S, BF16, "obf")
    Lb = [A.alloc((2, 2, 128), F32, "L%d" % i) for i in range(2)]
    Pm = [A.alloc((2, 2, 128), BF16, "Pm%d" % i) for i in range(2)]
    ui = 0
    for s in range(nseq):
        for hp in range(4):
            P.dma("sp", qh, d["qT"][s, hp], reads=[ctx.Rf("qT", s)], writes=[qhB])
            P.dma("sp", kh, d["kT"][s, hp], reads=[ctx.Rf("kT", s)], writes=[khB])
            P.dma("sp", bT, d["biasT"][hp], writes=[bTB])
            for b, (w, dd) in enumerate(BRANCHES):
                nb = 32 // dd
                vsrc = d["vv"][s].rearrange("(i r) c -> r i c", r=dd)
                for r in range(dd):
                    P.dma("sp", vraw[b][0][:, r * nb:(r + 1) * nb, :],
                          vsrc[r].rearrange("(n p) c -> p n c", p=128)[:, :, hp * 128:(hp + 1) * 128],
                          reads=[ctx.Rf("vv", s)], writes=[vraw[b][1]])
                P.add("pool", lambda e, b=b: e.tensor_copy(va0[b][0][:, :, 0:64], vraw[b][0][:, :, 0:64]),
                      reads=[vraw[b][1]], writes=[va0[b][1]])
                P.add("pool", lambda e, b=b: e.tensor_copy(v0b[b][0][:, :, 64:128], vraw[b][0][:, :, 64:128]),
                      reads=[vraw[b][1]], writes=[v0b[b][1]])
            for b, (w, dd) in enumerate(BRANCHES):
                nb = 32 // dd
                for r in range(dd):
                    for n in range(nb):
                        c0 = n * 128 * dd + r
                        qcols = slice(c0, c0 + 127 * dd + 1, dd)
                        pcols = slice(c0 - 128 * dd, c0 - dd + 1, dd)
                        kbs = (0, 1) if n > 0 else (1,)
                        L, LB = Lb[ui % 2]
                        pm, pmB = Pm[ui % 2]
                        k0 = kbs[0]
                        for h in range(2):
                            rows = slice(h * 64, (h + 1) * 64)
                            bk = (ui % 2) * 2 + h
                            ps, psB = PS[bk], PSB[bk]
                            psv = ps[:, 0:256].rearrange("p (k q) -> p k q", k=2)
                            for kb in kbs:
                                kc = pcols if kb == 0 else qcols
                                P.mm(psv[:, kb, :], kh[rows, kc], qh[rows, qcols], True, True,
                                     reads=[khB, qhB], writes=[psB])
                            P.add("dve", lambda e, L=L, psv=psv, b=b, k0=k0, h=h: e.tensor_tensor(
                                L[:, h, k0:2, :], psv[:, k0:2, :], bT[:, b, h, k0:2, :], ALU.add),
                                reads=[psB, bTB], writes=[LB])
                        P.act(pm[:, :, k0:2, :], L[:, :, k0:2, :], AF.Exp, reads=[LB], writes=[pmB])
                        ps2, ps2B = PS[4 + ui % 2], PSB[4 + ui % 2]
                        p2v = ps2[:, 0:256].rearrange("p (a q) -> p a q", a=2)
                        tile_c = r * nb + n
                        lst = [(h, kb) for h in range(2) for kb in kbs]
                        for i, (h, kb) in enumerate(lst):
                            vt = (va0 if h == 0 else v0b)[b]
                            P.mm(p2v[:, 0, :], vt[0][:, tile_c - 1 + kb, :], pm[:, h, kb, :], i == 0, i == len(lst) - 1,
                                 reads=[vt[1], pmB], writes=[ps2B])
                        for i, (h, kb) in enumerate(lst):
                            on = (onesA0 if h == 0 else ones0B)
                            P.mm(p2v[:, 1, :], on, pm[:, h, kb, :], i == 0, i == len(lst) - 1,
                                 reads=[onesA0B, ones0BB, pmB], writes=[ps2B])
                        if b == 0:
                            P.act(acc[:, :, qcols], p2v, AF.Identity, reads=[ps2B], writes=[accB])
                        else:
                            P.add("dve", lambda e, qcols=qcols, p2v=p2v: e.tensor_tensor(
                                acc[:, :, qcols], acc[:, :, qcols], p2v, ALU.add), reads=[ps2B, accB], writes=[accB])
                        ui += 1
            P.add("dve", lambda e: e.reciprocal(acc[:, 1, :], acc[:, 1, :]), reads=[accB], writes=[accB])
            P.add("dve", lambda e: e.tensor_tensor(obf, acc[:, 0, :], acc[:, 1, :], ALU.mult), reads=[accB], writes=[obfB])
            P.dma("sp", d["mixT"][s, hp], obf, reads=[obfB], writes=[ctx.Rf("mixT", s, 0)])


def phase_B_odd(ctx, layer):
    P, A, d, nseq = ctx.P, ctx.A, ctx.d, ctx.nseq
    PS, PSB = ctx.PS, ctx.PSB
    j = layer // 2
    P.barrier()
    A.reset()
    PADW = 32
    hc, hcB = A.alloc(PADW + S, F32, "hc")
    y4 = [A.alloc(S, F32, "y%d" % c) for c in range(4)]
    wdw, wdwB = A.alloc((4, 31), F32, "wdw")
    for c in range(4):
        P.dma("sp", wdw[:, c, :], d["od_w_dw"][j][:, c * 128:(c + 1) * 128].rearrange("j p -> p j"), writes=[wdwB],
              allow_slow_non_contiguous=True)
    bdw, bdwB = load_vec_fm(ctx, d["od_b_dw"][j], A, "bdw", 4)
    lg, lgB = load_vec_fm(ctx, d["od_conv_ln_g"][j], A, "clg", 4)
    lb, lbB = load_vec_fm(ctx, d["od_conv_ln_b"][j], A, "clb", 4)
    cones32, ccones32B = A.alloc(128, F32, "cones32")
    P.add("pool", lambda e: e.memset(cones32, 1.0), writes=[ccones32B])
    P.add("pool", lambda e: e.memset(hc[:, 0:PADW], 0.0), writes=[hcB])
    ysq = [A.alloc(512, F32, "ysq%d" % i) for i in range(2)]
    mean, meanB = A.alloc(512, F32, "cmean")
    msq, msqB = A.alloc(512, F32, "cmsq")
    rstd, rstdB = A.alloc(512, F32, "crstd")
    tmpc = [A.alloc(512, F32, "tmpc%d" % i) for i in range(2)]
    oc_st = [A.alloc((4, 512), BF16, "ocst%d" % i) for i in range(2)]
    for s in range(nseq):
        for c in range(4):
            P.dma("sp", hc[:, PADW:PADW + S], d["hcT"][s, c], reads=[ctx.Rf("hcT", s)], writes=[hcB])
            y, yB = y4[c]
            eng = "dve" if c % 2 == 0 else "dve"
            for tap in range(31):
                off = PADW - 30 + tap
                if tap == 0:
                    P.add(eng, lambda e, y=y, off=off, c=c: e.tensor_scalar(
                        y, hc[:, off:off + S], wdw[:, c, 0:1], bdw[:, c:c + 1], op0=ALU.mult, op1=ALU.add),
                        reads=[hcB, wdwB, bdwB], writes=[yB])
                else:
                    P.add(eng, lambda e, y=y, off=off, c=c, tap=tap: e.scalar_tensor_tensor(
                        y, hc[:, off:off + S], wdw[:, c, tap:tap + 1], y, ALU.mult, ALU.add),
                        reads=[hcB, wdwB, yB], writes=[yB])
        for blk in range(8):
            cols = slice(blk * 512, (blk + 1) * 512)
            ps1, ps1B = PS[0], PSB[0]
            ps2, ps2B = PS[1], PSB[1]
            for c in range(4):
                P.mm(ps1, cones32, y4[c][0][:, cols], c == 0, c == 3, reads=[ccones32B, y4[c][1]], writes=[ps1B])
            for c in range(4):
                q_, qB_ = ysq[c % 2]
                P.act(q_, y4[c][0][:, cols], AF.Square, reads=[y4[c][1]], writes=[qB_])
                P.mm(ps2, cones32, q_, c == 0, c == 3, reads=[ccones32B, qB_], writes=[ps2B])
            P.add("dve", lambda e, ps1=ps1: e.tensor_scalar(mean, ps1, 1.0 / 512, None, op0=ALU.mult), reads=[ps1B], writes=[meanB])
            P.add("dve", lambda e: e.tensor_tensor(msq, mean, mean, ALU.mult), reads=[meanB], writes=[msqB])
            P.add("dve", lambda e, ps2=ps2: e.scalar_tensor_tensor(msq, ps2, 1.0 / 512, msq, ALU.mult, ALU.subtract), reads=[ps2B, msqB], writes=[msqB])
            if ctx.debug and blk == 0 and s == 0:
                P.dma("sp", d["conv_dbg"][0], mean, reads=[meanB])
                P.dma("sp", d["conv_dbg"][1], msq, reads=[msqB])
                P.dma("sp", d["conv_dbg"][3], y4[0][0][:, 0:512], reads=[y4[0][1]])
                P.dma("sp", d["conv_dbg"][4], y4[3][0][:, 0:512], reads=[y4[3][1]])
            P.act(msq, msq, AF.Ln, reads=[msqB], writes=[msqB], bias=EPS)
            P.act(rstd, msq, AF.Exp, reads=[msqB], writes=[rstdB], scale=-0.5)
            if ctx.debug and blk == 0 and s == 0:
                P.dma("sp", d["conv_dbg"][2], rstd, reads=[rstdB])
            ost, ostB = oc_st[blk % 2]
            for c in range(4):
                t_, tB_ = tmpc[c % 2]
                P.add("dve", lambda e, t_=t_, c=c, cols=cols: e.tensor_tensor(t_, y4[c][0][:, cols], mean, ALU.subtract),
                      reads=[y4[c][1], meanB], writes=[tB_])
                P.add("pool", lambda e, t_=t_: e.tensor_tensor(t_, t_, rstd, ALU.mult), reads=[tB_, rstdB], writes=[tB_])
                P.act(ost[:, c, :], t_, AF.Silu, reads=[tB_, lgB, lbB], writes=[ostB], scale=lg[:, c:c + 1], bias=lb[:, c:c + 1])
            P.dma("sp", d["mixT"][s, 0:4].rearrange("h p t -> p h t")[:, :, cols], ost, reads=[ostB], writes=[ctx.Rf("mixT", s, 0)])

    P.barrier()
    A.reset()
    qh, qhB = A.alloc(S, BF16, "qh")
    kh, khB = A.alloc(S, BF16, "kh")
    nkh, nkhB = A.alloc(S, BF16, "nkh")
    vraw, vrawB = A.alloc((32, 128), BF16, "vraw")
    va0, va0B = A.alloc((32, 128), BF16, "va0")
    v0b, v0bB = A.alloc((32, 128), BF16, "v0b")
    P.add("pool", lambda e: e.memset(va0[:, :, 64:128], 0.0), writes=[va0B])
    P.add("pool", lambda e: e.memset(v0b[:, :, 0:64], 0.0), writes=[v0bB])
    mk, mkB = A.alloc((4, 512), F32, "sbmask")
    P.dma("sp", mk, d["sbmask"], writes=[mkB])
    mkb, mkbB = A.alloc((4, 512), BF16, "sbmaskb")
    P.add("dve", lambda e: e.tensor_copy(mkb, mk), reads=[mkB], writes=[mkbB])
    ut, utB = A.alloc(128, F32, "utri")
    P.dma("sp", ut, d["utri"], writes=[utB])
    ones32, ones32B = A.alloc(128, F32, "ones32")
    P.add("pool", lambda e: e.memset(ones32, 1.0), writes=[ones32B])
    e1 = [A.alloc(512, F32, "e1_%d" % i) for i in range(2)]
    sp32 = [A.alloc(512, F32, "sp32_%d" % i) for i in range(2)]
    att = [A.alloc(512, BF16, "att%d" % i) for i in range(2)]
    sacc = [A.alloc(512, F32, "sacc%d" % i) for i in range(2)]
    od, odB = A.alloc(S, BF16, "od")
    ui = 0
    for s in range(nseq):
        for hp in range(4):
            P.dma("sp", qh, d["qT"][s, hp], reads=[ctx.Rf("qT", s)], writes=[qhB])
            P.dma("sp", kh, d["kT"][s, hp], reads=[ctx.Rf("kT", s)], writes=[khB])
            P.add("pool", lambda e: e.tensor_scalar(nkh, kh, -1.0, None, op0=ALU.mult), reads=[khB], writes=[nkhB])
            P.dma("sp", vraw, d["vv"][s].rearrange("(n p) c -> p n c", p=128)[:, :, hp * 128:(hp + 1) * 128],
                  reads=[ctx.Rf("vv", s)], writes=[vrawB])
            P.add("pool", lambda e: e.tensor_copy(va0[:, :, 0:64], vraw[:, :, 0:64]), reads=[vrawB], writes=[va0B])
            P.add("pool", lambda e: e.tensor_copy(v0b[:, :, 64:128], vraw[:, :, 64:128]), reads=[vrawB], writes=[v0bB])
            for Q in range(8):
                qcols = slice(Q * 512, (Q + 1) * 512)
                pso, psoB = PS[6 + Q % 2], PSB[6 + Q % 2]
                nkb = 4 * Q + 4
                for ki, kb in enumerate(range(nkb - 1, -1, -1)):
                    kcols = slice(kb * 128, (kb + 1) * 128)
                    diag = kb >= 4 * Q
                    for h in range(2):
                        rows = slice(h * 64, (h + 1) * 64)
                        psz, pszB = PS[ui % 2], PSB[ui % 2]
                        psc, pscB = PS[2 + ui % 2], PSB[2 + ui % 2]
                        e_, eB_ = e1[ui % 2]
                        sp_, spB_ = sp32[ui % 2]
                        at_, atB_ = att[ui % 2]
                        sa_, saB_ = sacc[h]
                        P.mm(psz, kh[rows, kcols], qh[rows, qcols], True, True, reads=[khB, qhB], writes=[pszB])
                        P.act(e_, psz, AF.Exp, reads=[pszB], writes=[eB_])
                        P.act(sp_, e_, AF.Ln, reads=[eB_], writes=[spB_], bias=1.0)
                        if diag:
                            i = kb - 4 * Q
                            P.add("pool", lambda e, sp_=sp_, i=i: e.tensor_tensor(sp_, sp_, mk[:, i, :], ALU.mult),
                                  reads=[spB_, mkB], writes=[spB_])
                        P.mm(psc, ut, sp_, True, False, reads=[utB, spB_], writes=[pscB])
                        if ki > 0:
                            P.mm(psc, ones32, sa_, False, False, reads=[ones32B, saB_], writes=[pscB])
                        P.mm(psc, nkh[rows, kcols], qh[rows, qcols], False, True, reads=[nkhB, qhB], writes=[pscB])
                        P.act(at_, psc, AF.Exp, reads=[pscB], writes=[atB_], scale=-1.0)
                        if diag:
                            P.add("pool", lambda e, at_=at_, i=i: e.tensor_tensor(at_, at_, mkb[:, i, :], ALU.mult),
                                  reads=[atB_, mkbB], writes=[atB_])
                        vt, vtB = (va0, va0B) if h == 0 else (v0b, v0bB)
                        P.mm(pso, vt[:, kb, :], at_, ki == 0 and h == 0, kb == 0 and h == 1,
                             reads=[vtB, atB_], writes=[psoB])
                        if ki == 0:
                            P.add("dve", lambda e, sa_=sa_, sp_=sp_: e.tensor_copy(sa_, sp_), reads=[spB_], writes=[saB_])
                        elif kb > 0:
                            P.add("dve", lambda e, sa_=sa_, sp_=sp_: e.tensor_tensor(sa_, sa_, sp_, ALU.add),
                                  reads=[spB_, saB_], writes=[saB_])
                        ui += 1
                P.add("dve", lambda e, pso=pso, qcols=qcols: e.tensor_copy(od[:, qcols], pso), reads=[psoB], writes=[odB])
            P.dma("sp", d["mixT"][s, 4 + hp], od, reads=[odB], writes=[ctx.Rf("mixT", s, 1)])


def phase_C(ctx, layer):
    P, A, d, nseq = ctx.P, ctx.A, ctx.d, ctx.nseq
    PS, PSB = ctx.PS, ctx.PSB
    even = layer % 2 == 0
    j = layer // 2
    P.barrier()
    A.reset()
    wout, woutB = A.alloc((8, D), BF16, "wout")
    P.dma("pool", wout, d["ev_w_out" if even else "od_w_out"][j].rearrange("(c p) n -> p c n", p=128), writes=[woutB])
    wr, wrB = A.alloc((8, 20), F32, "wr")
    P.dma("sp", wr, d["moe_wr"][layer].rearrange("(c p) n -> p c n", p=128), writes=[wrB])
    brb, brbB = A.alloc((4, 20), F32, "brb")
    for t in range(4):
        src = bass.AP(d["moe_br"].tensor, d["moe_br"][layer].offset, [[0, 128], [1, 20]])
        P.dma("sp", brb[:, t, :], src, writes=[brbB])
    scr = {k: A.alloc(4, F32, k) for k in ("ss", "lnv", "rstd")}
    scr["junk"] = A.alloc(1024, BF16, "junk")
    xb = [A.alloc((4, 1024), F32, "xt%d" % i) for i in range(2)]
    xn, xnB = A.alloc((4, 1024), F32, "xn")
    mxb = [A.alloc((8, 512), BF16, "mx%d" % i) for i in range(2)]
    hTb = [A.alloc((8, 512), BF16, "hT%d" % i) for i in range(2)]
    h32 = [A.alloc(512, F32, "h32_%d" % i) for i in range(2)]
    tmp = [A.alloc(512, F32, "tmp%d" % i) for i in range(2)]
    lg, lgB = A.alloc((4, 20), F32, "lg")
    small = {k: A.alloc(4, F32, "r_" + k) for k in ("gmax", "nmax", "gsum", "gw", "m1", "m2", "dm", "e21", "w1", "w2")}
    big = {k: A.alloc((4, 4), F32, "r_" + k) for k in ("ex", "oh", "el", "mask1", "el2", "mask2", "gsel")}
    bi = 0
    for s in range(nseq):
        gs, gsB, sh, shB = make_gs_sh(ctx, layer, s, 3, 4, d["norm2_g"][layer], A, "2_%d" % s)
        g1b, g1bB = load_bcast(ctx, d["mod"][layer, s, 2 * D:3 * D], D, A, "g1b%d" % s, reads=[ctx.Rf("mod")])
        for blk in range(8):
            t0 = s * S + blk * 512
            cols = slice(blk * 512, (blk + 1) * 512)
            xt, xtB = xb[bi % 2]
            mx, mxB = mxb[bi % 2]
            hT, hTB = hTb[bi % 2]
            src_x = d["x"] if layer == 0 else d["xs"]
            rd = [] if layer == 0 else [ctx.Rf("xs", s, blk)]
            P.dma("sp", xt, src_x[t0:t0 + 512, :].rearrange("(t p) dd -> p t dd", p=128), reads=rd, writes=[xtB])
            P.dma("sp", mx, d["mixT"][s].rearrange("h p t -> p h t")[:, :, cols],
                  reads=[ctx.Rf("mixT", s, 0), ctx.Rf("mixT", s, 1)], writes=[mxB])
            k = 0
            for t in range(4):
                for half in range(2):
                    ps, psB = PS[4 + k % 4], PSB[4 + k % 4]
                    hs = slice(half * 512, (half + 1) * 512)
                    for c in range(8):
                        P.mm(ps, mx[:, c, t * 128:(t + 1) * 128], wout[:, c, hs], c == 0, c == 7, reads=[mxB, woutB], writes=[psB])
                    tm, tmB = tmp[k % 2]
                    P.add("dve", lambda e, tm=tm, ps=ps, hs=hs, g1b=g1b: e.tensor_tensor(tm, ps, g1b[:, hs], ALU.mult),
                          reads=[psB, g1bB], writes=[tmB])
                    P.add("pool", lambda e, tm=tm, xt=xt, t=t, hs=hs: e.tensor_tensor(xt[:, t, hs], xt[:, t, hs], tm, ALU.add),
                          reads=[tmB, xtB], writes=[xtB])
                    k += 1
            P.dma("sp", d["xs"][t0:t0 + 512, :].rearrange("(t p) dd -> p t dd", p=128), xt, reads=[xtB], writes=[ctx.Rf("xs", s, blk)])
            P.add("pool", lambda e, xt=xt: e.tensor_copy(xn, xt), reads=[xtB], writes=[xnB])
            psr, psrB = PS[2], PSB[2]
            psrv = psr[:, 0:128].rearrange("p (t n) -> p t n", t=4)

            def router(c, h, hB, psrv=psrv, psrB=psrB):
                for t in range(4):
                    P.mm(psrv[:, t, 0:20], h[:, t * 128:(t + 1) * 128], wr[:, c, :], c == 0 and t == 0, c == 7,
                         reads=[hB, wrB], writes=[psrB])
            nmt(ctx, xn, xnB, gs, gsB, sh, shB, hT, hTB, scr, h32=h32, router=router)
            P.dma("sp", d["h2T"][s].rearrange("h p t -> p h t")[:, :, cols], hT, reads=[hTB], writes=[ctx.Rf("h2T", s, blk // 4)])
            V = lambda k_: small[k_][0]
            VB = lambda k_: small[k_][1]
            W = lambda k_: big[k_][0]
            WB = lambda k_: big[k_][1]
            dv = lambda fn, rd_, wr_: P.add("dve", fn, reads=rd_, writes=wr_)
            dv(lambda e, psrv=psrv: e.tensor_tensor(lg, psrv[:, :, 0:20], brb, ALU.add), [psrB, brbB], [lgB])
            dv(lambda e: e.tensor_reduce(V("gmax"), lg[:, :, 0:4], AX.X, ALU.max), [lgB], [VB("gmax")])
            dv(lambda e: e.tensor_scalar(V("nmax"), V("gmax"), -1.0, None, op0=ALU.mult), [VB("gmax")], [VB("nmax")])
            P.add("pool", lambda e: e.memset(V("gsum"), 0.0), writes=[VB("gsum")])
            for t in range(4):
                P.act(W("ex")[:, t, :], lg[:, t, 0:4], AF.Exp, reads=[lgB, VB("nmax")], writes=[WB("ex"), VB("gsum")],
                      bias=V("nmax")[:, t:t + 1], accum_out=V("gsum")[:, t:t + 1])
            dv(lambda e: e.reciprocal(V("gw"), V("gsum")), [VB("gsum")], [VB("gw")])
            for t in range(4):
                dv(lambda e, t=t: e.tensor_scalar(W("oh")[:, t, :], lg[:, t, 0:4], V("gmax")[:, t:t + 1], None, op0=ALU.is_equal),
                   [lgB, VB("gmax")], [WB("oh")])
            for t in range(4):
                for g in range(4):
                    src = lg[:, t, 4 + 4 * g:8 + 4 * g]
                    if g == 0:
                        dv(lambda e, t=t, src=src: e.tensor_scalar(W("el")[:, t, :], src, W("oh")[:, t, 0:1], None, op0=ALU.mult),
                           [lgB, WB("oh")], [WB("el")])
                    else:
                        dv(lambda e, t=t, g=g, src=src: e.scalar_tensor_tensor(W("el")[:, t, :], src, W("oh")[:, t, g:g + 1], W("el")[:, t, :], ALU.mult, ALU.add),
                           [lgB, WB("oh"), WB("el")], [WB("el")])
            dv(lambda e: e.tensor_reduce(V("m1"), W("el"), AX.X, ALU.max), [WB("el")], [VB("m1")])
            for t in range(4):
                dv(lambda e, t=t: e.tensor_scalar(W("mask1")[:, t, :], W("el")[:, t, :], V("m1")[:, t:t + 1], None, op0=ALU.is_equal),
                   [WB("el"), VB("m1")], [WB("mask1")])
            dv(lambda e: e.scalar_tensor_tensor(W("el2"), W("mask1"), -1e30, W("el"), ALU.mult, ALU.add), [WB("mask1"), WB("el")], [WB("el2")])
            dv(lambda e: e.tensor_reduce(V("m2"), W("el2"), AX.X, ALU.max), [WB("el2")], [VB("m2")])
            for t in range(4):
                dv(lambda e, t=t: e.tensor_scalar(W("mask2")[:, t, :], W("el2")[:, t, :], V("m2")[:, t:t + 1], None, op0=ALU.is_equal),
                   [WB("el2"), VB("m2")], [WB("mask2")])
            dv(lambda e: e.tensor_tensor(V("dm"), V("m2"), V("m1"), ALU.subtract), [VB("m1"), VB("m2")], [VB("dm")])
            P.act(V("e21"), V("dm"), AF.Exp, reads=[VB("dm")], writes=[VB("e21")])
            dv(lambda e: e.tensor_scalar(V("w1"), V("e21"), 1.0, None, op0=ALU.add), [VB("e21")], [VB("w1")])
            dv(lambda e: e.reciprocal(V("w1"), V("w1")), [VB("w1")], [VB("w1")])
            dv(lambda e: e.tensor_tensor(V("w2"), V("e21"), V("w1"), ALU.mult), [VB("e21"), VB("w1")], [VB("w2")])
            dv(lambda e: e.tensor_tensor(V("w1"), V("w1"), V("gw"), ALU.mult), [VB("w1"), VB("gw")], [VB("w1")])
            dv(lambda e: e.tensor_tensor(V("w2"), V("w2"), V("gw"), ALU.mult), [VB("w2"), VB("gw")], [VB("w2")])
            for t in range(4):
                dv(lambda e, t=t: e.tensor_scalar(W("gsel")[:, t, :], W("mask1")[:, t, :], V("w1")[:, t:t + 1], None, op0=ALU.mult),
                   [WB("mask1"), VB("w1")], [WB("gsel")])
                dv(lambda e, t=t: e.scalar_tensor_tensor(W("gsel")[:, t, :], W("mask2")[:, t, :], V("w2")[:, t:t + 1], W("gsel")[:, t, :], ALU.mult, ALU.add),
                   [WB("mask2"), VB("w2"), WB("gsel")], [WB("gsel")])
            for t in range(4):
                tile_g = s * 32 + blk * 4 + t
                for g in range(4):
                    dv(lambda e, t=t, g=g, tile_g=tile_g: e.tensor_scalar(
                        ctx.gates[:, tile_g, 4 * g:4 * g + 4], W("gsel")[:, t, :], W("oh")[:, t, g:g + 1], None, op0=ALU.mult),
                       [WB("gsel"), WB("oh")], [ctx.gatesB])
            if ctx.debug:
                P.dma("sp", d["gates_dbg"][t0:t0 + 512, :].rearrange("(t p) n -> p t n", p=128),
                      ctx.gates[:, s * 32 + blk * 4:s * 32 + blk * 4 + 4, :], reads=[ctx.gatesB])
            bi += 1


def phase_D(ctx, layer):
    P, A, d, nseq = ctx.P, ctx.A, ctx.d, ctx.nseq
    PS, PSB = ctx.PS, ctx.PSB
    last = layer == DEPTH - 1
    P.barrier()
    A.reset()
    SB = 2048
    nsb = nseq * S // SB
    hT, hTB = A.alloc((8, SB), BF16, "hT")
    yacc, yaccB = A.alloc((16, D), F32, "yacc")
    wts = []
    for i in range(2):
        wts.append((A.alloc((8, 512), BF16, "wg%d" % i), A.alloc((8, 512), BF16, "wu%d" % i), A.alloc((4, D), BF16, "wd%d" % i)))
    hid = [A.alloc((4, 512), BF16, "hid%d" % i) for i in range(2)]
    sg = [A.alloc(512, F32, "sg%d" % i) for i in range(2)]
    xtb = [A.alloc(D, F32, "xt%d" % i) for i in range(2)]
    ss, ssB = A.alloc(1, F32, "fss")
    junk, junkB = A.alloc(D, BF16, "fjunk")
    if last:
        gfb, gfbB = load_bcast(ctx, d["final_norm_g"], D, A, "gfb")
    ei = 0
    for sb in range(nsb):
        s = sb // 2
        half_s = sb % 2
        g2b, g2bB = load_bcast(ctx, d["mod"][layer, s, 5 * D:6 * D], D, A, "g2b%d" % sb, reads=[ctx.Rf("mod")])
        P.dma("sp", hT, d["h2T"][s].rearrange("h p t -> p h t")[:, :, half_s * SB:(half_s + 1) * SB],
              reads=[ctx.Rf("h2T", s, half_s)], writes=[hTB])
        for ex in range(16):
            (wg, wgB), (wu, wuB), (wd, wdB) = wts[ei % 2]
            P.dma("pool", wg, d["moe_w_gate"][layer, ex].rearrange("(c p) n -> p c n", p=128), writes=[wgB])
            P.dma("pool", wu, d["moe_w_up"][layer, ex].rearrange("(c p) n -> p c n", p=128), writes=[wuB])
            P.dma("pool", wd, d["moe_w_down"][layer, ex].rearrange("(c p) n -> p c n", p=128), writes=[wdB])
            ei += 1
            for blk in range(4):
                cols = slice(blk * 512, (blk + 1) * 512)
                hd, hdB = hid[blk % 2]
                for fc in range(4):
                    psg, psgB = PS[fc % 2], PSB[fc % 2]
                    psu, psuB = PS[2 + fc % 2], PSB[2 + fc % 2]
                    for c in range(8):
                        P.mm(psg, wg[:, c, fc * 128:(fc + 1) * 128], hT[:, c, cols], c == 0, c == 7, reads=[wgB, hTB], writes=[psgB])
                    for c in range(8):
                        P.mm(psu, wu[:, c, fc * 128:(fc + 1) * 128], hT[:, c, cols], c == 0, c == 7, reads=[wuB, hTB], writes=[psuB])
                    sg_, sgB_ = sg[fc % 2]
                    P.act(sg_, psg, AF.Silu, reads=[psgB], writes=[sgB_])
                    P.add("dve", lambda e, hd=hd, fc=fc, sg_=sg_, psu=psu: e.tensor_tensor(hd[:, fc, :], sg_, psu, ALU.mult),
                          reads=[sgB_, psuB], writes=[hdB])
                k = 0
                for t in range(4):
                    tl = blk * 4 + t
                    tile_g = sb * 16 + tl
                    for half in range(2):
                        hs = slice(half * 512, (half + 1) * 512)
                        psy, psyB = PS[4 + k % 4], PSB[4 + k % 4]
                        k += 1
                        for fc in range(4):
                            P.mm(psy, hd[:, fc, t * 128:(t + 1) * 128], wd[:, fc, hs], fc == 0, fc == 3, reads=[hdB, wdB], writes=[psyB])
                        if ex == 0:
                            P.add("dve", lambda e, tl=tl, hs=hs, psy=psy, tile_g=tile_g, ex=ex: e.tensor_scalar(
                                yacc[:, tl, hs], psy, ctx.gates[:, tile_g, ex:ex + 1], None, op0=ALU.mult),
                                reads=[psyB, ctx.gatesB], writes=[yaccB])
                        else:
                            P.add("dve", lambda e, tl=tl, hs=hs, psy=psy, tile_g=tile_g, ex=ex: e.scalar_tensor_tensor(
                                yacc[:, tl, hs], psy, ctx.gates[:, tile_g, ex:ex + 1], yacc[:, tl, hs], ALU.mult, ALU.add),
                                reads=[psyB, ctx.gatesB, yaccB], writes=[yaccB])
        if ctx.debug and layer == 0:
            P.dma("sp", d["yacc_dbg"][sb * SB:(sb + 1) * SB, :].rearrange("(t p) n -> p t n", p=128), yacc, reads=[yaccB])
        for tl in range(16):
            t0 = sb * SB + tl * 128
            blk_g = (t0 % S) // 512
            xt, xtB = xtb[tl % 2]
            P.dma("sp", xt, d["xs"][t0:t0 + 128, :], reads=[ctx.Rf("xs", s, blk_g)], writes=[xtB])
            P.add("pool", lambda e, tl=tl, g2b=g2b: e.tensor_tensor(yacc[:, tl, :], yacc[:, tl, :], g2b, ALU.mult), reads=[yaccB, g2bB], writes=[yaccB])
            P.add("pool", lambda e, tl=tl, xt=xt: e.tensor_tensor(xt, xt, yacc[:, tl, :], ALU.add), reads=[yaccB, xtB], writes=[xtB])
            if not last:
                P.dma("sp", d["xs"][t0:t0 + 128, :], xt, reads=[xtB], writes=[ctx.Rf("xs", s, blk_g)])
            else:
                P.add("pool", lambda e: e.memset(ss, 0.0), writes=[ssB])
                P.act(junk, xt, AF.Square, reads=[xtB], writes=[junkB, ssB], accum_out=ss)
                P.act(ss, ss, AF.Ln, reads=[ssB], writes=[ssB], scale=1.0 / D, bias=EPS)
                P.act(ss, ss, AF.Exp, reads=[ssB], writes=[ssB], scale=-0.5)
                P.add("dve", lambda e, xt=xt: e.tensor_scalar(xt, xt, ss[:, 0:1], None, op0=ALU.mult), reads=[xtB, ssB], writes=[xtB])
                P.add("dve", lambda e, xt=xt: e.tensor_tensor(xt, xt, gfb, ALU.mult), reads=[xtB, gfbB], writes=[xtB])
                P.dma("sp", d["out"][t0:t0 + 128, :], xt, reads=[xtB])


def prep_shared(inp):
    f = lambda a: np.ascontiguousarray(np.asarray(a, dtype=np.float32))
    sh = {}
    sh["biasT"] = _bias_tables(f(inp["rel_bias"]))
    for k in ("norm1_g", "norm2_g", "w_ada", "b_ada", "ev_w_in", "ev_w_out", "ev_gmlp_ln_g", "ev_gmlp_ln_b",
              "ev_b_s", "od_w_in", "od_w_out", "od_w_dw", "od_b_dw", "od_conv_ln_g", "od_conv_ln_b",
              "moe_w_gate", "moe_w_up", "moe_w_down", "final_norm_g"):
        sh[k] = f(inp[k])
    sh["ev_w_sT"] = f(np.transpose(np.asarray(inp["ev_w_s"]), (0, 1, 3, 2)))
    wg = np.asarray(inp["moe_w_group"])
    wrt = np.asarray(inp["moe_w_router"])
    sh["moe_wr"] = f(np.concatenate([wg, np.transpose(wrt, (0, 2, 1, 3)).reshape(DEPTH, D, 16)], axis=2))
    sh["moe_br"] = f(np.concatenate([np.asarray(inp["moe_b_group"]), np.asarray(inp["moe_b_router"]).reshape(DEPTH, 16)], axis=1))
    sh.update(_consts())
    return sh


def core_inputs(inp, shared, seqs):
    m = dict(shared)
    m["x"] = np.ascontiguousarray(np.asarray(inp["x"], dtype=np.float32)[seqs].reshape(len(seqs) * S, D))
    m["c"] = np.ascontiguousarray(np.asarray(inp["c"], dtype=np.float32)[seqs])
    return m


_CACHE = {}


def kernel(**inputs):
    nseq = 2
    if "nc" not in _CACHE:
        _CACHE["nc"] = build(nseq)[0]
    nc = _CACHE["nc"]
    shared = prep_shared(inputs)
    in_maps = [core_inputs(inputs, shared, list(range(i * nseq, (i + 1) * nseq))) for i in range(NCORES)]
    res = run_bass_kernel_spmd(nc, in_maps, core_ids=list(range(NCORES)))
    out = np.concatenate([np.asarray(r["out"]).reshape(nseq, S, D) for r in res.results], axis=0)
    return out.astype(np.float32)
```

```python
import numpy as np
import concourse.bass as bass
import concourse.mybir as mybir
from concourse.bass_utils import run_bass_kernel_spmd
from contextlib import ExitStack

F32 = mybir.dt.float32
BF16 = mybir.dt.bfloat16
AF = mybir.ActivationFunctionType
ALU = mybir.AluOpType
AX = mybir.AxisListType

D = 1024
S = 4096
DEPTH = 4
NCORES = 8
EPS = 1e-6
NEG = -1e30
BRANCHES = ((128, 1), (512, 4), (2048, 16))

ENGS = ("pe", "act", "dve", "pool", "sp")
DMA_K = 12


class Buf:
    __slots__ = ("name", "lw", "rd", "lws")

    def __init__(self, name=""):
        self.name = name
        self.lw = None
        self.rd = []
        self.lws = []


class Op:
    __slots__ = ("eng", "fn", "deps", "pos", "tick", "dma", "dj", "need")


class Prog:
    def __init__(self, nc):
        self.nc = nc
        self.ops = {e: [] for e in ENGS}
        self.ndma = {e: 0 for e in ENGS}
        self.pending_dma = []

    def add(self, eng, fn, reads=(), writes=(), dma=False, extra_deps=None):
        op = Op()
        op.eng = eng
        op.fn = fn
        op.dma = dma
        op.pos = len(self.ops[eng])
        op.tick = None
        op.need = False
        op.dj = None
        deps = set()
        for b in reads:
            for w_ in b.lws:
                deps.add(w_)
        for b in writes:
            for w_ in b.lws:
                deps.add(w_)
            for r in b.rd:
                deps.add(r)
        if extra_deps:
            deps |= set(extra_deps)
        deps.discard(op)
        op.deps = deps
        if dma:
            op.dj = self.ndma[eng]
            self.ndma[eng] += 1
            self.pending_dma.append(op)
        wset = set(id(b) for b in writes)
        for b in writes:
            if dma and b.lws and all(w_.dma for w_ in b.lws):
                b.lws = b.lws[-24:] + [op]
            else:
                b.lws = [op]
            b.lw = op
            b.rd = []
        for b in reads:
            if id(b) in wset:
                continue
            if not dma:
                b.rd = [r for r in b.rd if r.dma or r.eng != eng]
            b.rd.append(op)
        self.ops[eng].append(op)
        return op

    def barrier(self):
        deps = set(self.pending_dma)
        for e in ENGS:
            for o in reversed(self.ops[e]):
                if o.fn is not None and not o.dma:
                    deps.add(o)
                    break
        for e in ENGS:
            self.add(e, None, extra_deps=deps)
        self.pending_dma = []

    def dma(self, q, out, in_, reads=(), writes=(), **kw):
        return self.add(q, lambda e: e.dma_start(out=out, in_=in_, **kw), reads, writes, dma=True)

    def mm(self, out, lhsT, rhs, start, stop, reads=(), writes=(), **kw):
        return self.add("pe", lambda e: e.matmul(out, lhsT, rhs, start=start, stop=stop, **kw),
                        reads, writes)

    def tr(self, out, in_, ident, reads=(), writes=()):
        return self.add("pe", lambda e: e.transpose(out, in_, ident), reads, writes)

    def act(self, out, in_, func, reads=(), writes=(), **kw):
        return self.add("act", lambda e: e.activation(out, in_, func, **kw), reads, writes)

    def _hazard(self, op, d):
        if d.dma or op.dma:
            return True
        if op.eng == "pe":
            return False
        return d.pos >= op.pos - 2

    def emit(self, stack):
        nc = self.nc
        sem = {e: stack.enter_context(nc.semaphore("s_" + e)) for e in ENGS}
        dsem = {e: [stack.enter_context(nc.semaphore("d_%s_%d" % (e, i))) for i in range(DMA_K)]
                for e in ENGS if self.ndma[e] > 0}
        for e in ENGS:
            for op in self.ops[e]:
                for d in op.deps:
                    if d.dma:
                        continue
                    if d.eng != e or self._hazard(op, d):
                        d.need = True
        for e in ENGS:
            t = 0
            for op in self.ops[e]:
                if op.dma or op.fn is None:
                    continue
                if op.need:
                    t += 1
                    op.tick = t
        self.nticks = {e: sum(1 for o in self.ops[e] if o.tick) for e in ENGS}
        block = stack.enter_context(nc.Block())
        nwaits = {e: 0 for e in ENGS}

        def run(e, eng):
            seen = {x: 0 for x in ENGS}
            dseen = {}
            for op in self.ops[e]:
                waits = {}
                dwaits = {}
                for d in op.deps:
                    if d.dma:
                        key = (d.eng, d.dj % DMA_K)
                        val = 16 * (d.dj // DMA_K + 1)
                        if dseen.get(key, 0) < val:
                            dwaits[key] = max(dwaits.get(key, 0), val)
                    else:
                        if d.fn is None:
                            continue
                        if d.eng == e and not self._hazard(op, d):
                            continue
                        if seen[d.eng] < d.tick:
                            waits[d.eng] = max(waits.get(d.eng, 0), d.tick)
                if op.dma and op.dj >= DMA_K:
                    key = (e, op.dj % DMA_K)
                    val = 16 * (op.dj // DMA_K)
                    if dseen.get(key, 0) < val:
                        dwaits[key] = max(dwaits.get(key, 0), val)
                for x, v in waits.items():
                    eng.wait_ge(sem[x], v)
                    seen[x] = v
                    nwaits[e] += 1
                for key, v in dwaits.items():
                    eng.wait_ge(dsem[key[0]][key[1]], v)
                    dseen[key] = v
                    nwaits[e] += 1
                if op.fn is None:
                    continue
                ins = op.fn(eng)
                if op.dma:
                    ins.then_inc(dsem[e][op.dj % DMA_K], 16)
                elif op.tick is not None:
                    ins.then_inc(sem[e], 1)
            if self.ndma[e] > 0:
                n = self.ndma[e]
                for i in range(min(DMA_K, n)):
                    cnt = (n - 1 - i) // DMA_K + 1
                    eng.wait_ge(dsem[e][i], 16 * cnt)

        @block.tensor
        def _(eng):
            run("pe", eng)

        @block.scalar
        def _(eng):
            run("act", eng)

        @block.vector
        def _(eng):
            run("dve", eng)

        @block.gpsimd
        def _(eng):
            run("pool", eng)

        @block.sync
        def _(eng):
            run("sp", eng)

        self.nwaits = nwaits


class Arena:
    def __init__(self, nc, st, nbytes):
        self.t16 = st.enter_context(nc.sbuf_tensor("arena", [128, nbytes // 2], BF16))
        self.t32 = self.t16.bitcast(F32)
        self.nbytes = nbytes
        self.off = 0
        self.floor = 0

    def reset(self):
        self.off = self.floor

    def alloc(self, free, dt, name=""):
        if isinstance(free, int):
            free = (free,)
        n = 1
        for f in free:
            n *= f
        es = 2 if dt == BF16 else 4
        off = (self.off + 63) // 64 * 64
        self.off = off + n * es
        assert self.off <= self.nbytes, ("SBUF arena overflow", name, self.off)
        base = self.t16 if dt == BF16 else self.t32
        eo = off // es
        ap = base[:, eo:eo + n]
        if len(free) == 2:
            ap = ap.rearrange("p (a b) -> p a b", b=free[1])
        elif len(free) == 3:
            ap = ap.rearrange("p (a b c) -> p a b c", b=free[1], c=free[2])
        elif len(free) == 4:
            ap = ap.rearrange("p (a b c d) -> p a b c d", b=free[1], c=free[2], d=free[3])
        return ap, Buf(name)


def _t5_bucket(dist):
    n_buckets, max_distance = 32, 2048
    max_exact = n_buckets // 2
    d = np.maximum(dist, 1).astype(np.float32)
    large = max_exact + (np.log(d / max_exact) / np.log(max_distance / max_exact)
                         * (n_buckets - max_exact)).astype(np.int32)
    large = np.minimum(large, n_buckets - 1)
    return np.where(dist < max_exact, dist, large).astype(np.int32)


def _bias_tables(rel_bias):
    out = np.full((4, 128, 3, 2, 2, 128), NEG, np.float32)
    k = np.arange(128)[:, None]
    q = np.arange(128)[None, :]
    for bi, (w, d) in enumerate(BRANCHES):
        for kb in range(2):
            sub = q + 128 - (k + 128 * kb)
            valid = (sub >= 0) & (sub <= w // d)
            bucket = _t5_bucket(np.clip(sub, 0, None) * d)
            for hp in range(4):
                for h in range(2):
                    g = rel_bias[bucket, hp * 2 + h]
                    out[hp, :, bi, h, kb, :] = np.where(valid, g, np.float32(NEG))
    return out


def _consts():
    c = {}
    c["ident"] = np.eye(128, dtype=np.float32)
    k = np.arange(128)[:, None]
    q = np.arange(512)[None, :]
    sb = np.stack([((i * 128 + k) < q).astype(np.float32) for i in range(4)], 1)
    c["sbmask"] = sb
    c["utri"] = (np.arange(128)[:, None] >= np.arange(128)[None, :]).astype(np.float32)
    c["tril_st"] = (np.arange(128)[:, None] <= np.arange(128)[None, :]).astype(np.float32)
    return c


class Ctx:
    pass


def build(nseq, stop_after=None, debug=False):
    nc = bass.Bass("TRN2", target_bir_lowering=False)
    T = nseq * S
    ctx = Ctx()
    ctx.nc = nc
    ctx.nseq = nseq
    ctx.debug = debug
    d = {}

    def din(name, shape, dt=F32):
        d[name] = nc.dram_tensor(name, list(shape), dt, kind="ExternalInput").ap()

    def dscr(name, shape, dt):
        kind = "ExternalOutput" if debug else "Internal"
        d[name] = nc.dram_tensor(name, list(shape), dt, kind=kind).ap()

    din("x", (T, D))
    din("c", (nseq, D))
    din("biasT", (4, 128, 3, 2, 2, 128))
    din("norm1_g", (DEPTH, D))
    din("norm2_g", (DEPTH, D))
    din("w_ada", (DEPTH, D, 6 * D))
    din("b_ada", (DEPTH, 6 * D))
    din("ev_w_in", (2, D, 2560))
    din("ev_w_out", (2, D, D))
    din("ev_gmlp_ln_g", (2, 512))
    din("ev_gmlp_ln_b", (2, 512))
    din("ev_w_sT", (2, 4, 128, 128))
    din("ev_b_s", (2, 4, 128))
    din("od_w_in", (2, D, 2560))
    din("od_w_out", (2, D, D))
    din("od_w_dw", (2, 31, 512))
    din("od_b_dw", (2, 512))
    din("od_conv_ln_g", (2, 512))
    din("od_conv_ln_b", (2, 512))
    din("moe_wr", (DEPTH, D, 20))
    din("moe_br", (DEPTH, 20))
    din("moe_w_gate", (DEPTH, 16, D, 512))
    din("moe_w_up", (DEPTH, 16, D, 512))
    din("moe_w_down", (DEPTH, 16, 512, D))
    din("final_norm_g", (D,))
    din("ident", (128, 128))
    din("sbmask", (128, 4, 512))
    din("utri", (128, 128))
    din("tril_st", (128, 128))
    d["out"] = nc.dram_tensor("out", [T, D], F32, kind="ExternalOutput").ap()
    dscr("xs", (T, D), F32)
    dscr("mod", (DEPTH, nseq, 6 * D), F32)
    dscr("qT", (nseq, 4, 128, S), BF16)
    dscr("kT", (nseq, 4, 128, S), BF16)
    dscr("vv", (nseq, S, 512), BF16)
    dscr("mixT", (nseq, 8, 128, S), BF16)
    dscr("hcT", (nseq, 4, 128, S), F32)
    dscr("h2T", (nseq, 8, 128, S), BF16)
    if debug:
        dscr("gates_dbg", (T, 16), F32)
        dscr("yacc_dbg", (T, D), F32)
        dscr("conv_dbg", (5, 128, 512), F32)
    ctx.d = d
    ctx.R = {}

    def R(name, *key):
        k = (name,) + key
        if k not in ctx.R:
            ctx.R[k] = Buf(str(k))
        return ctx.R[k]
    ctx.Rf = R

    P = Prog(nc)
    ctx.P = P
    with ExitStack() as st:
        A = Arena(nc, st, 206 * 1024)
        ctx.A = A
        ctx.PS = []
        ctx.PSB = []
        for i in range(8):
            ctx.PS.append(st.enter_context(nc.psum_tensor("ps%d" % i, [128, 512], F32))[:, :])
            ctx.PSB.append(Buf("ps%d" % i))
        ctx.ident, ctx.identB = A.alloc(128, F32, "ident")
        P.dma("sp", ctx.ident, d["ident"], writes=[ctx.identB])
        ctx.gates, ctx.gatesB = A.alloc((nseq * 32, 16), F32, "gates")
        A.floor = A.off

        phases = [("mods", None)]
        for l in range(DEPTH):
            phases += [("A", l), ("B", l), ("C", l), ("D", l)]
        for ph in phases:
            if ph[0] == "mods":
                phase_mods(ctx)
            elif ph[0] == "A":
                phase_A(ctx, ph[1])
            elif ph[0] == "B":
                if ph[1] % 2 == 0:
                    phase_B_even(ctx, ph[1])
                else:
                    phase_B_odd(ctx, ph[1])
            elif ph[0] == "C":
                phase_C(ctx, ph[1])
            elif ph[0] == "D":
                phase_D(ctx, ph[1])
            if stop_after is not None and ph == stop_after:
                break
        P.emit(st)
    ctx.prog = P
    return nc, ctx


def phase_mods(ctx):
    P, A, d, nseq = ctx.P, ctx.A, ctx.d, ctx.nseq
    P.barrier()
    A.reset()
    cT, cTB = A.alloc((8, nseq), F32, "cT")
    sc, scB = A.alloc((8, nseq), F32, "sc")
    for s_ in range(nseq):
        P.dma("sp", cT[:, :, s_], d["c"][s_].rearrange("(c p) -> p c", p=128), writes=[cTB],
              allow_slow_non_contiguous=True)
    P.act(sc, cT, AF.Silu, reads=[cTB], writes=[scB])
    bada, badaB = A.alloc(6 * D, F32, "bada")
    modsb, modsbB = A.alloc(6 * D, F32, "modsb")
    wb = [A.alloc((8, 512), F32, "wada%d" % i) for i in range(3)]
    i = 0
    for l in range(DEPTH):
        for s in range(nseq):
            P.dma("sp", bada[s:s + 1], d["b_ada"][l:l + 1, :], writes=[badaB])
        for cb in range(12):
            w, wB = wb[i % 3]
            P.dma("sp", w, d["w_ada"][l].rearrange("(c p) n -> p c n", p=128)[:, :, cb * 512:(cb + 1) * 512],
                  writes=[wB])
            ps, psB = ctx.PS[i % 2], ctx.PSB[i % 2]
            for c in range(8):
                P.mm(ps[0:nseq, :], sc[:, c, :], w[:, c, :], c == 0, c == 7, reads=[scB, wB], writes=[psB])
            P.add("dve", lambda e, ps=ps, cb=cb: e.tensor_tensor(
                modsb[0:nseq, cb * 512:(cb + 1) * 512], ps[0:nseq, :],
                bada[0:nseq, cb * 512:(cb + 1) * 512], ALU.add),
                reads=[psB, badaB], writes=[modsbB])
            i += 1
        P.dma("sp", d["mod"][l], modsb[0:nseq], reads=[modsbB], writes=[ctx.Rf("mod")])


def load_mod_fm(ctx, layer, s, which, A, name):
    P, d = ctx.P, ctx.d
    t, tB = A.alloc(8, F32, name)
    src = d["mod"][layer, s, which * D:(which + 1) * D].rearrange("(c p) -> p c", p=128)
    P.dma("sp", t, src, reads=[ctx.Rf("mod")], writes=[tB], allow_slow_non_contiguous=True)
    return t, tB


def load_vec_fm(ctx, vec_ap, A, name, n=8):
    P = ctx.P
    t, tB = A.alloc(n, F32, name)
    P.dma("sp", t, vec_ap.rearrange("(c p) -> p c", p=128), writes=[tB], allow_slow_non_contiguous=True)
    return t, tB


def load_bcast(ctx, vec_ap, n, A, name, reads=()):
    P = ctx.P
    t, tB = A.alloc(n, F32, name)
    src = bass.AP(vec_ap.tensor, vec_ap.offset, [[0, 128], [1, n]])
    P.dma("sp", t, src, reads=list(reads), writes=[tB])
    return t, tB


def make_gs_sh(ctx, layer, s, which_sh, which_sc, gvec_ap, A, tag):
    P = ctx.P
    sh, shB = load_mod_fm(ctx, layer, s, which_sh, A, "sh" + tag)
    scv, scB = load_mod_fm(ctx, layer, s, which_sc, A, "sc" + tag)
    g, gB = load_vec_fm(ctx, gvec_ap, A, "g" + tag)
    gs, gsB = A.alloc(8, F32, "gs" + tag)
    P.add("dve", lambda e: e.scalar_tensor_tensor(gs, scv, 1.0, g, ALU.add, ALU.mult),
          reads=[scB, gB], writes=[gsB])
    return gs, gsB, sh, shB


def nmt_s1(ctx, xt, xtB, scr):
    P = ctx.P
    ss, ssB = scr["ss"]
    lnv, lnvB = scr["lnv"]
    rstd, rstdB = scr["rstd"]
    junk, junkB = scr["junk"]
    P.add("pool", lambda e: e.memset(ss, 0.0), writes=[ssB])
    for t in range(4):
        P.act(junk, xt[:, t, :], AF.Square, reads=[xtB], writes=[junkB, ssB], accum_out=ss[:, t:t + 1])
    P.act(lnv, ss, AF.Ln, reads=[ssB], writes=[lnvB], scale=1.0 / D, bias=EPS)
    P.act(rstd, lnv, AF.Exp, reads=[lnvB], writes=[rstdB], scale=-0.5)
    for t in range(4):
        P.add("dve", lambda e, t=t: e.tensor_scalar(xt[:, t, :], xt[:, t, :], rstd[:, t:t + 1], None, op0=ALU.mult),
              reads=[xtB, rstdB], writes=[xtB])


def nmt_s2(ctx, xt, xtB, gs, gsB, sh, shB, hT, hTB, h32=None, router=None):
    P = ctx.P
    for c in range(8):
        ps, psB = ctx.PS[c % 2], ctx.PSB[c % 2]
        for t in range(4):
            P.tr(ps[:, t * 128:(t + 1) * 128], xt[:, t, c * 128:(c + 1) * 128], ctx.ident,
                 reads=[xtB, ctx.identB], writes=[psB])
        if h32 is None:
            P.act(hT[:, c, :], ps, AF.Identity, reads=[psB, gsB, shB], writes=[hTB],
                  scale=gs[:, c:c + 1], bias=sh[:, c:c + 1])
        else:
            h, hB = h32[c % 2]
            P.act(h, ps, AF.Identity, reads=[psB, gsB, shB], writes=[hB],
                  scale=gs[:, c:c + 1], bias=sh[:, c:c + 1])
            P.add("dve", lambda e, h=h, c=c: e.tensor_copy(hT[:, c, :], h), reads=[hB], writes=[hTB])
            router(c, h, hB)


def pipeline3(n, s1, s2, s3):
    s1(0)
    s2(0)
    for b in range(n):
        if b + 1 < n:
            s1(b + 1)
        s3(b)
        if b + 1 < n:
            s2(b + 1)


def phase_A(ctx, layer):
    P, A, d, nseq = ctx.P, ctx.A, ctx.d, ctx.nseq
    even = layer % 2 == 0
    j = layer // 2
    P.barrier()
    A.reset()
    PS, PSB = ctx.PS, ctx.PSB
    win, winB = A.alloc((8, 2560), BF16, "win")
    P.dma("pool", win, d["ev_w_in" if even else "od_w_in"][j].rearrange("(c p) n -> p c n", p=128), writes=[winB])
    scr = {k: A.alloc(4, F32, k) for k in ("ss", "lnv", "rstd")}
    scr["junk"] = A.alloc(1024, BF16, "junk")
    xb = [A.alloc((4, 1024), F32, "xt%d" % i) for i in range(2)]
    hTb = [A.alloc((8, 512), BF16, "hT%d" % i) for i in range(2)]
    qst = [A.alloc((4, 512), BF16, "qst%d" % i) for i in range(2)]
    kst = [A.alloc((4, 512), BF16, "kst%d" % i) for i in range(2)]
    vst = [A.alloc((4, 512), BF16, "vst%d" % i) for i in range(2)]
    if even:
        wsf, wsfB = A.alloc((4, 128), F32, "wsf")
        P.dma("sp", wsf, d["ev_w_sT"][j].rearrange("g s t -> s g t"), writes=[wsfB])
        tril, trilB = A.alloc(128, F32, "tril")
        P.dma("sp", tril, d["tril_st"], writes=[trilB])
        wsm, wsmB = A.alloc((4, 128), BF16, "wsm")
        for g in range(4):
            P.add("dve", lambda e, g=g: e.tensor_tensor(wsm[:, g, :], wsf[:, g, :], tril, ALU.mult),
                  reads=[wsfB, trilB], writes=[wsmB])
        bsf, bsfB = A.alloc(512, F32, "bsf")
        P.dma("sp", bsf[0:1], d["ev_b_s"][j].rearrange("(o g) t -> o (g t)", o=1), writes=[bsfB])
        bsr, bsrB = A.alloc(512, BF16, "bsr")
        onesr, onesrB = A.alloc(128, BF16, "onesr")
        P.add("dve", lambda e: e.tensor_copy(bsr[0:1], bsf[0:1]), reads=[bsfB], writes=[bsrB])
        P.add("dve", lambda e: e.memset(onesr[0:1], 1.0), writes=[onesrB])
        lng, lngB = load_bcast(ctx, d["ev_gmlp_ln_g"][j], 512, A, "lng")
        lnb, lnbB = load_bcast(ctx, d["ev_gmlp_ln_b"][j], 512, A, "lnb")
        ug = [A.alloc((4, 512), BF16, "ug%d" % i) for i in range(2)]
        gg = [A.alloc((4, 512), F32, "gg%d" % i) for i in range(2)]
        vg = [A.alloc((4, 512), BF16, "vg%d" % i) for i in range(2)]
        obst = [A.alloc((4, 512), BF16, "obst%d" % i) for i in range(2)]
        st1 = [A.alloc(4, F32, "s1_%d" % i) for i in range(2)]
        st2 = [A.alloc(4, F32, "s2_%d" % i) for i in range(2)]
        mean = A.alloc(4, F32, "mean")
        msq = A.alloc(4, F32, "msq")
        var = A.alloc(4, F32, "var")
        rs2 = A.alloc(4, F32, "rs2")
    else:
        hcst = [A.alloc((4, 512), F32, "hcst%d" % i) for i in range(2)]
        sg = [A.alloc(512, F32, "sg%d" % i) for i in range(2)]
    src_x = d["x"] if layer == 0 else d["xs"]
    scr2 = [scr, {k: A.alloc(4, F32, k + "b") for k in ("ss", "lnv", "rstd")}]
    scr2[1]["junk"] = scr["junk"]
    gsd = {}
    blocks = [(s, blk) for s in range(nseq) for blk in range(8)]

    def s1(bi):
        s, blk = blocks[bi]
        if blk == 0:
            gsd[s] = make_gs_sh(ctx, layer, s, 0, 1, d["norm1_g"][layer], A, "1_%d" % s)
        t0 = s * S + blk * 512
        xt, xtB = xb[bi % 2]
        rd = [] if layer == 0 else [ctx.Rf("xs", s, blk)]
        P.dma("sp", xt, src_x[t0:t0 + 512, :].rearrange("(t p) dd -> p t dd", p=128), reads=rd, writes=[xtB])
        nmt_s1(ctx, xt, xtB, scr2[bi % 2])

    def s2(bi):
        s, blk = blocks[bi]
        gs, gsB, sh, shB = gsd[s]
        xt, xtB = xb[bi % 2]
        hT, hTB = hTb[bi % 2]
        nmt_s2(ctx, xt, xtB, gs, gsB, sh, shB, hT, hTB)

    def s3(bi):
        s, blk = blocks[bi]
        cols = slice(blk * 512, (blk + 1) * 512)
        hT, hTB = hTb[bi % 2]
        qs, qsB = qst[bi % 2]
        ks, ksB = kst[bi % 2]
        vs, vsB = vst[bi % 2]
        if even:
            qcol, kcol, vcol = 0, 512, 1024
        else:
            qcol, kcol, vcol = 1024, 1536, 2048
        n_fm = 0
        for oc in range(8):
            col0 = (qcol if oc < 4 else kcol) + (oc % 4) * 128
            ps, psB = PS[2 + n_fm % 2], PSB[2 + n_fm % 2]
            n_fm += 1
            for c in range(8):
                P.mm(ps, win[:, c, col0:col0 + 128], hT[:, c, :], c == 0, c == 7, reads=[winB, hTB], writes=[psB])
            if oc < 4:
                P.act(qs[:, oc, :], ps, AF.Identity, reads=[psB], writes=[qsB], scale=0.125)
            else:
                P.add("dve", lambda e, ps=ps, oc=oc, ks=ks: e.tensor_copy(ks[:, oc - 4, :], ps), reads=[psB], writes=[ksB])
        P.dma("sp", d["qT"][s].rearrange("h p t -> p h t")[:, :, cols], qs, reads=[qsB], writes=[ctx.Rf("qT", s)])
        P.dma("sp", d["kT"][s].rearrange("h p t -> p h t")[:, :, cols], ks, reads=[ksB], writes=[ctx.Rf("kT", s)])
        if even:
            u, uB = ug[bi % 2]
            for oc in range(4):
                col0 = 1536 + oc * 128
                ps, psB = PS[2 + n_fm % 2], PSB[2 + n_fm % 2]
                n_fm += 1
                for c in range(8):
                    P.mm(ps, win[:, c, col0:col0 + 128], hT[:, c, :], c == 0, c == 7, reads=[winB, hTB], writes=[psB])
                P.act(u[:, oc, :], ps, AF.Gelu_apprx_tanh, reads=[psB], writes=[uB])
        else:
            hc, hcB = hcst[bi % 2]
            for oc in range(4):
                psa, psaB = PS[2 + 4 * (oc % 2)], PSB[2 + 4 * (oc % 2)]
                psg, psgB = PS[3 + 4 * (oc % 2)], PSB[3 + 4 * (oc % 2)]
                for c in range(8):
                    P.mm(psa, win[:, c, oc * 128:(oc + 1) * 128], hT[:, c, :], c == 0, c == 7, reads=[winB, hTB], writes=[psaB])
                for c in range(8):
                    P.mm(psg, win[:, c, 512 + oc * 128:512 + (oc + 1) * 128], hT[:, c, :], c == 0, c == 7, reads=[winB, hTB], writes=[psgB])
                sgt, sgtB = sg[oc % 2]
                P.act(sgt, psg, AF.Sigmoid, reads=[psgB], writes=[sgtB])
                P.add("dve", lambda e, psa=psa, sgt=sgt, hc=hc, oc=oc: e.tensor_tensor(hc[:, oc, :], psa, sgt, ALU.mult),
                      reads=[psaB, sgtB], writes=[hcB])
            P.dma("sp", d["hcT"][s].rearrange("h p t -> p h t")[:, :, cols], hc, reads=[hcB], writes=[ctx.Rf("hcT", s)])
        if even:
            s1, s1B = st1[bi % 2]
            s2, s2B = st2[bi % 2]
            vgt, vgB = vg[bi % 2]
            g4, g4B = gg[bi % 2]
            P.add("pool", lambda e, s1=s1: e.memset(s1, 0.0), writes=[s1B])
            P.add("pool", lambda e, s2=s2: e.memset(s2, 0.0), writes=[s2B])
        for t in range(4):
            ps, psB = PS[4 + t % 2], PSB[4 + t % 2]
            for c in range(8):
                P.mm(ps, hT[:, c, t * 128:(t + 1) * 128], win[:, c, vcol:vcol + 512], c == 0, c == 7, reads=[winB, hTB], writes=[psB])
            P.add("dve", lambda e, ps=ps, t=t, vs=vs: e.tensor_copy(vs[:, t, :], ps), reads=[psB], writes=[vsB])
            if even:
                ps2, ps2B = PS[6 + t % 2], PSB[6 + t % 2]
                for c in range(8):
                    P.mm(ps2, hT[:, c, t * 128:(t + 1) * 128], win[:, c, 2048:2560], c == 0, c == 7, reads=[winB, hTB], writes=[ps2B])
                P.act(g4[:, t, :], ps2, AF.Gelu_apprx_tanh, reads=[ps2B], writes=[g4B, s1B], accum_out=s1[:, t:t + 1])
                P.act(scr["junk"][0][:, 0:512], g4[:, t, :], AF.Square, reads=[g4B], writes=[scr["junk"][1], s2B], accum_out=s2[:, t:t + 1])
        P.dma("sp", d["vv"][s, blk * 512:(blk + 1) * 512, :].rearrange("(t p) n -> p t n", p=128), vs, reads=[vsB], writes=[ctx.Rf("vv", s)])
        if even:
            mn, mnB = mean
            mq, mqB = msq
            vr, vrB = var
            r2, r2B = rs2
            P.add("dve", lambda e, s1=s1, mn=mn: e.tensor_scalar(mn, s1, 1.0 / 512, None, op0=ALU.mult), reads=[s1B], writes=[mnB])
            P.add("dve", lambda e, mn=mn, mq=mq: e.tensor_tensor(mq, mn, mn, ALU.mult), reads=[mnB], writes=[mqB])
            P.add("dve", lambda e, s2=s2, mq=mq, vr=vr: e.scalar_tensor_tensor(vr, s2, 1.0 / 512, mq, ALU.mult, ALU.subtract), reads=[s2B, mqB], writes=[vrB])
            P.act(vr, vr, AF.Ln, reads=[vrB], writes=[vrB], bias=EPS)
            P.act(r2, vr, AF.Exp, reads=[vrB], writes=[r2B], scale=-0.5)
            for t in range(4):
                P.add("dve", lambda e, t=t, g4=g4, mn=mn, r2=r2: e.tensor_scalar(
                    g4[:, t, :], g4[:, t, :], mn[:, t:t + 1], r2[:, t:t + 1], op0=ALU.subtract, op1=ALU.mult),
                    reads=[g4B, mnB, r2B], writes=[g4B])
                P.add("pool", lambda e, t=t, g4=g4: e.tensor_tensor(g4[:, t, :], g4[:, t, :], lng, ALU.mult),
                      reads=[g4B, lngB], writes=[g4B])
                P.add("dve", lambda e, t=t, g4=g4, vgt=vgt: e.tensor_tensor(vgt[:, t, :], g4[:, t, :], lnb, ALU.add),
                      reads=[g4B, lnbB], writes=[vgB])
            ob, obB = obst[bi % 2]
            for g in range(4):
                ps, psB = PS[2 + g % 2], PSB[2 + g % 2]
                for t in range(4):
                    P.mm(ps[:, t * 128:(t + 1) * 128], vgt[:, t, g * 128:(g + 1) * 128], wsm[:, g, :], True, False,
                         reads=[vgB, wsmB], writes=[psB])
                    P.mm(ps[:, t * 128:(t + 1) * 128], onesr[0:1, :], bsr[0:1, g * 128:(g + 1) * 128], False, True,
                         reads=[onesrB, bsrB], writes=[psB])
                P.add("dve", lambda e, ps=ps, g=g, ob=ob, u=u: e.tensor_tensor(ob[:, g, :], ps, u[:, g, :], ALU.mult),
                      reads=[psB, uB], writes=[obB])
            P.dma("sp", d["mixT"][s, 4:8].rearrange("h p t -> p h t")[:, :, cols], ob, reads=[obB], writes=[ctx.Rf("mixT", s, 1)])


    pipeline3(len(blocks), s1, s2, s3)


def phase_B_even(ctx, layer):
    P, A, d, nseq = ctx.P, ctx.A, ctx.d, ctx.nseq
    PS, PSB = ctx.PS, ctx.PSB
    P.barrier()
    A.reset()
    qh, qhB = A.alloc(S, BF16, "qh")
    kh, khB = A.alloc(S, BF16, "kh")
    vraw = [A.alloc((32, 128), BF16, "vraw%d" % b) for b in range(3)]
    va0 = [A.alloc((32, 128), BF16, "va0_%d" % b) for b in range(3)]
    v0b = [A.alloc((32, 128), BF16, "v0b_%d" % b) for b in range(3)]
    onesA0, onesA0B = A.alloc(128, BF16, "onesA0")
    ones0B, ones0BB = A.alloc(128, BF16, "ones0B")
    P.add("pool", lambda e: e.memset(onesA0[:, 0:64], 1.0), writes=[onesA0B])
    P.add("pool", lambda e: e.memset(onesA0[:, 64:128], 0.0), writes=[onesA0B])
    P.add("pool", lambda e: e.memset(ones0B[:, 0:64], 0.0), writes=[ones0BB])
    P.add("pool", lambda e: e.memset(ones0B[:, 64:128], 1.0), writes=[ones0BB])
    for b in range(3):
        P.add("pool", lambda e, b=b: e.memset(va0[b][0][:, :, 64:128], 0.0), writes=[va0[b][1]])
        P.add("pool", lambda e, b=b: e.memset(v0b[b][0][:, :, 0:64], 0.0), writes=[v0b[b][1]])
    bT, bTB = A.alloc((3, 2, 2, 128), F32, "biasT")
    acc, accB = A.alloc((2, S), F32, "acc")
    obf, obfB = A.alloc(S, BF16, "obf")
    Lb = [A.alloc((2, 2, 128), F32, "L%d" % i) for i in range(2)]
    Pm = [A.alloc((2, 2, 128), BF16, "Pm%d" % i) for i in range(2)]
    ui = 0
    for s in range(nseq):
        for hp in range(4):
            P.dma("sp", qh, d["qT"][s, hp], reads=[ctx.Rf("qT", s)], writes=[qhB])
            P.dma("sp", kh, d["kT"][s, hp], reads=[ctx.Rf("kT", s)], writes=[khB])
            P.dma("sp", bT, d["biasT"][hp], writes=[bTB])
            for b, (w, dd) in enumerate(BRANCHES):
                nb = 32 // dd
                vsrc = d["vv"][s].rearrange("(i r) c -> r i c", r=dd)
                for r in range(dd):
                    P.dma("sp", vraw[b][0][:, r * nb:(r + 1) * nb, :],
                          vsrc[r].rearrange("(n p) c -> p n c", p=128)[:, :, hp * 128:(hp + 1) * 128],
                          reads=[ctx.Rf("vv", s)], writes=[vraw[b][1]])
                P.add("pool", lambda e, b=b: e.tensor_copy(va0[b][0][:, :, 0:64], vraw[b][0][:, :, 0:64]),
                      reads=[vraw[b][1]], writes=[va0[b][1]])
                P.add("pool", lambda e, b=b: e.tensor_copy(v0b[b][0][:, :, 64:128], vraw[b][0][:, :, 64:128]),
                      reads=[vraw[b][1]], writes=[v0b[b][1]])
            units = []
            for b, (w, dd) in enumerate(BRANCHES):
                nb = 32 // dd
                for r in range(dd):
                    for n in range(nb):
                        units.append((b, dd, nb, r, n))

            def stage1(u, ui):
                b, dd, nb, r, n = u
                c0 = n * 128 * dd + r
                qcols = slice(c0, c0 + 127 * dd + 1, dd)
                pcols = slice(c0 - 128 * dd, c0 - dd + 1, dd)
                kbs = (0, 1) if n > 0 else (1,)
                L, LB = Lb[ui % 2]
                pm, pmB = Pm[ui % 2]
                k0 = kbs[0]
                for h in range(2):
                    rows = slice(h * 64, (h + 1) * 64)
                    bk = (ui % 2) * 2 + h
                    ps, psB = PS[bk], PSB[bk]
                    psv = ps[:, 0:256].rearrange("p (k q) -> p k q", k=2)
                    for kb in kbs:
                        kc = pcols if kb == 0 else qcols
                        P.mm(psv[:, kb, :], kh[rows, kc], qh[rows, qcols], True, True,
                             reads=[khB, qhB], writes=[psB])
                    P.add("dve", lambda e, L=L, psv=psv, b=b, k0=k0, h=h: e.tensor_tensor(
                        L[:, h, k0:2, :], psv[:, k0:2, :], bT[:, b, h, k0:2, :], ALU.add),
                        reads=[psB, bTB], writes=[LB])
                P.act(pm[:, :, k0:2, :], L[:, :, k0:2, :], AF.Exp, reads=[LB], writes=[pmB])

            def stage2(u, ui):
                b, dd, nb, r, n = u
                c0 = n * 128 * dd + r
                qcols = slice(c0, c0 + 127 * dd + 1, dd)
                kbs = (0, 1) if n > 0 else (1,)
                pm, pmB = Pm[ui % 2]
                ps2, ps2B = PS[4 + ui % 2], PSB[4 + ui % 2]
                p2v = ps2[:, 0:256].rearrange("p (a q) -> p a q", a=2)
                tile_c = r * nb + n
                lst = [(h, kb) for h in range(2) for kb in kbs]
                for i, (h, kb) in enumerate(lst):
                    vt = (va0 if h == 0 else v0b)[b]
                    P.mm(p2v[:, 0, :], vt[0][:, tile_c - 1 + kb, :], pm[:, h, kb, :], i == 0, i == len(lst) - 1,
                         reads=[vt[1], pmB], writes=[ps2B])
                for i, (h, kb) in enumerate(lst):
                    on = (onesA0 if h == 0 else ones0B)
                    P.mm(p2v[:, 1, :], on, pm[:, h, kb, :], i == 0, i == len(lst) - 1,
                         reads=[onesA0B, ones0BB, pmB], writes=[ps2B])
                if b == 0:
                    P.act(acc[:, :, qcols], p2v, AF.Identity, reads=[ps2B], writes=[accB])
                else:
                    P.add("dve", lambda e, qcols=qcols, p2v=p2v: e.tensor_tensor(
                        acc[:, :, qcols], acc[:, :, qcols], p2v, ALU.add), reads=[ps2B, accB], writes=[accB])

            stage1(units[0], 0)
            for ui in range(len(units)):
                if ui + 1 < len(units):
                    stage1(units[ui + 1], ui + 1)
                stage2(units[ui], ui)
            P.add("dve", lambda e: e.reciprocal(acc[:, 1, :], acc[:, 1, :]), reads=[accB], writes=[accB])
            P.add("dve", lambda e: e.tensor_tensor(obf, acc[:, 0, :], acc[:, 1, :], ALU.mult), reads=[accB], writes=[obfB])
            P.dma("sp", d["mixT"][s, hp], obf, reads=[obfB], writes=[ctx.Rf("mixT", s, 0)])


def phase_B_odd(ctx, layer):
    P, A, d, nseq = ctx.P, ctx.A, ctx.d, ctx.nseq
    PS, PSB = ctx.PS, ctx.PSB
    j = layer // 2
    P.barrier()
    A.reset()
    PADW = 32
    hc, hcB = A.alloc(PADW + S, F32, "hc")
    y4 = [A.alloc(S, F32, "y%d" % c) for c in range(4)]
    wdw, wdwB = A.alloc((4, 31), F32, "wdw")
    for c in range(4):
        P.dma("sp", wdw[:, c, :], d["od_w_dw"][j][:, c * 128:(c + 1) * 128].rearrange("j p -> p j"), writes=[wdwB],
              allow_slow_non_contiguous=True)
    bdw, bdwB = load_vec_fm(ctx, d["od_b_dw"][j], A, "bdw", 4)
    lg, lgB = load_vec_fm(ctx, d["od_conv_ln_g"][j], A, "clg", 4)
    lb, lbB = load_vec_fm(ctx, d["od_conv_ln_b"][j], A, "clb", 4)
    cones32, ccones32B = A.alloc(128, F32, "cones32")
    P.add("pool", lambda e: e.memset(cones32, 1.0), writes=[ccones32B])
    P.add("pool", lambda e: e.memset(hc[:, 0:PADW], 0.0), writes=[hcB])
    ysq = [A.alloc(512, F32, "ysq%d" % i) for i in range(2)]
    mean, meanB = A.alloc(512, F32, "cmean")
    msq, msqB = A.alloc(512, F32, "cmsq")
    rstd, rstdB = A.alloc(512, F32, "crstd")
    tmpc = [A.alloc(512, F32, "tmpc%d" % i) for i in range(2)]
    oc_st = [A.alloc((4, 512), BF16, "ocst%d" % i) for i in range(2)]
    for s in range(nseq):
        for c in range(4):
            P.dma("sp", hc[:, PADW:PADW + S], d["hcT"][s, c], reads=[ctx.Rf("hcT", s)], writes=[hcB])
            y, yB = y4[c]
            eng = "dve" if c % 2 == 0 else "dve"
            for tap in range(31):
                off = PADW - 30 + tap
                if tap == 0:
                    P.add(eng, lambda e, y=y, off=off, c=c: e.tensor_scalar(
                        y, hc[:, off:off + S], wdw[:, c, 0:1], bdw[:, c:c + 1], op0=ALU.mult, op1=ALU.add),
                        reads=[hcB, wdwB, bdwB], writes=[yB])
                else:
                    P.add(eng, lambda e, y=y, off=off, c=c, tap=tap: e.scalar_tensor_tensor(
                        y, hc[:, off:off + S], wdw[:, c, tap:tap + 1], y, ALU.mult, ALU.add),
                        reads=[hcB, wdwB, yB], writes=[yB])
        for blk in range(8):
            cols = slice(blk * 512, (blk + 1) * 512)
            ps1, ps1B = PS[0], PSB[0]
            ps2, ps2B = PS[1], PSB[1]
            for c in range(4):
                P.mm(ps1, cones32, y4[c][0][:, cols], c == 0, c == 3, reads=[ccones32B, y4[c][1]], writes=[ps1B])
            for c in range(4):
                q_, qB_ = ysq[c % 2]
                P.act(q_, y4[c][0][:, cols], AF.Square, reads=[y4[c][1]], writes=[qB_])
                P.mm(ps2, cones32, q_, c == 0, c == 3, reads=[ccones32B, qB_], writes=[ps2B])
            P.add("dve", lambda e, ps1=ps1: e.tensor_scalar(mean, ps1, 1.0 / 512, None, op0=ALU.mult), reads=[ps1B], writes=[meanB])
            P.add("dve", lambda e: e.tensor_tensor(msq, mean, mean, ALU.mult), reads=[meanB], writes=[msqB])
            P.add("dve", lambda e, ps2=ps2: e.scalar_tensor_tensor(msq, ps2, 1.0 / 512, msq, ALU.mult, ALU.subtract), reads=[ps2B, msqB], writes=[msqB])
            if ctx.debug and blk == 0 and s == 0:
                P.dma("sp", d["conv_dbg"][0], mean, reads=[meanB])
                P.dma("sp", d["conv_dbg"][1], msq, reads=[msqB])
                P.dma("sp", d["conv_dbg"][3], y4[0][0][:, 0:512], reads=[y4[0][1]])
                P.dma("sp", d["conv_dbg"][4], y4[3][0][:, 0:512], reads=[y4[3][1]])
            P.act(msq, msq, AF.Ln, reads=[msqB], writes=[msqB], bias=EPS)
            P.act(rstd, msq, AF.Exp, reads=[msqB], writes=[rstdB], scale=-0.5)
            if ctx.debug and blk == 0 and s == 0:
                P.dma("sp", d["conv_dbg"][2], rstd, reads=[rstdB])
            ost, ostB = oc_st[blk % 2]
            for c in range(4):
                t_, tB_ = tmpc[c % 2]
                P.add("dve", lambda e, t_=t_, c=c, cols=cols: e.tensor_tensor(t_, y4[c][0][:, cols], mean, ALU.subtract),
                      reads=[y4[c][1], meanB], writes=[tB_])
                P.add("pool", lambda e, t_=t_: e.tensor_tensor(t_, t_, rstd, ALU.mult), reads=[tB_, rstdB], writes=[tB_])
                P.act(ost[:, c, :], t_, AF.Silu, reads=[tB_, lgB, lbB], writes=[ostB], scale=lg[:, c:c + 1], bias=lb[:, c:c + 1])
            P.dma("sp", d["mixT"][s, 0:4].rearrange("h p t -> p h t")[:, :, cols], ost, reads=[ostB], writes=[ctx.Rf("mixT", s, 0)])

    P.barrier()
    A.reset()
    qh, qhB = A.alloc(S, BF16, "qh")
    kh, khB = A.alloc(S, BF16, "kh")
    nkh, nkhB = A.alloc(S, BF16, "nkh")
    vraw, vrawB = A.alloc((32, 128), BF16, "vraw")
    va0, va0B = A.alloc((32, 128), BF16, "va0")
    v0b, v0bB = A.alloc((32, 128), BF16, "v0b")
    P.add("pool", lambda e: e.memset(va0[:, :, 64:128], 0.0), writes=[va0B])
    P.add("pool", lambda e: e.memset(v0b[:, :, 0:64], 0.0), writes=[v0bB])
    mk, mkB = A.alloc((4, 512), F32, "sbmask")
    P.dma("sp", mk, d["sbmask"], writes=[mkB])
    mkb, mkbB = A.alloc((4, 512), BF16, "sbmaskb")
    P.add("dve", lambda e: e.tensor_copy(mkb, mk), reads=[mkB], writes=[mkbB])
    ut, utB = A.alloc(128, F32, "utri")
    P.dma("sp", ut, d["utri"], writes=[utB])
    ones32, ones32B = A.alloc(128, F32, "ones32")
    P.add("pool", lambda e: e.memset(ones32, 1.0), writes=[ones32B])
    e1 = [A.alloc(512, F32, "e1_%d" % i) for i in range(2)]
    spb = [A.alloc(512, BF16, "spb_%d" % i) for i in range(2)]
    utb, utbB = A.alloc(128, BF16, "utrib")
    P.add("dve", lambda e: e.tensor_copy(utb, ut), reads=[utB], writes=[utbB])
    att = [A.alloc(512, BF16, "att%d" % i) for i in range(2)]
    sacc = [A.alloc(512, F32, "sacc%d" % i) for i in range(2)]
    od, odB = A.alloc(S, BF16, "od")
    ui = 0
    for s in range(nseq):
        for hp in range(4):
            P.dma("sp", qh, d["qT"][s, hp], reads=[ctx.Rf("qT", s)], writes=[qhB])
            P.dma("sp", kh, d["kT"][s, hp], reads=[ctx.Rf("kT", s)], writes=[khB])
            P.add("pool", lambda e: e.tensor_scalar(nkh, kh, -1.0, None, op0=ALU.mult), reads=[khB], writes=[nkhB])
            P.dma("sp", vraw, d["vv"][s].rearrange("(n p) c -> p n c", p=128)[:, :, hp * 128:(hp + 1) * 128],
                  reads=[ctx.Rf("vv", s)], writes=[vrawB])
            P.add("pool", lambda e: e.tensor_copy(va0[:, :, 0:64], vraw[:, :, 0:64]), reads=[vrawB], writes=[va0B])
            P.add("pool", lambda e: e.tensor_copy(v0b[:, :, 64:128], vraw[:, :, 64:128]), reads=[vrawB], writes=[v0bB])
            units = []
            for Q in range(8):
                nkb = 4 * Q + 4
                for ki, kb in enumerate(range(nkb - 1, -1, -1)):
                    for h in range(2):
                        units.append((Q, ki, kb, h))

            def stage1(u, ui):
                Q, ki, kb, h = u
                qcols = slice(Q * 512, (Q + 1) * 512)
                kcols = slice(kb * 128, (kb + 1) * 128)
                rows = slice(h * 64, (h + 1) * 64)
                psz, pszB = PS[ui % 2], PSB[ui % 2]
                e_, eB_ = e1[ui % 2]
                sp_, spB_ = spb[ui % 2]
                P.mm(psz, kh[rows, kcols], qh[rows, qcols], True, True, reads=[khB, qhB], writes=[pszB])
                P.act(e_, psz, AF.Exp, reads=[pszB], writes=[eB_])
                P.act(sp_, e_, AF.Ln, reads=[eB_], writes=[spB_], bias=1.0)
                if kb >= 4 * Q:
                    i = kb - 4 * Q
                    P.add("pool", lambda e, sp_=sp_, i=i: e.tensor_tensor(sp_, sp_, mkb[:, i, :], ALU.mult),
                          reads=[spB_, mkbB], writes=[spB_])

            def stage2(u, ui):
                Q, ki, kb, h = u
                qcols = slice(Q * 512, (Q + 1) * 512)
                kcols = slice(kb * 128, (kb + 1) * 128)
                rows = slice(h * 64, (h + 1) * 64)
                pso, psoB = PS[6 + Q % 2], PSB[6 + Q % 2]
                psc, pscB = PS[2 + ui % 2], PSB[2 + ui % 2]
                sp_, spB_ = spb[ui % 2]
                at_, atB_ = att[ui % 2]
                sa_, saB_ = sacc[h]
                P.mm(psc, utb, sp_, True, False, reads=[utbB, spB_], writes=[pscB])
                if ki > 0:
                    P.mm(psc, ones32, sa_, False, False, reads=[ones32B, saB_], writes=[pscB])
                P.mm(psc, nkh[rows, kcols], qh[rows, qcols], False, True, reads=[nkhB, qhB], writes=[pscB])
                P.act(at_, psc, AF.Exp, reads=[pscB], writes=[atB_], scale=-1.0)
                if kb >= 4 * Q:
                    i = kb - 4 * Q
                    P.add("pool", lambda e, at_=at_, i=i: e.tensor_tensor(at_, at_, mkb[:, i, :], ALU.mult),
                          reads=[atB_, mkbB], writes=[atB_])
                vt, vtB = (va0, va0B) if h == 0 else (v0b, v0bB)
                P.mm(pso, vt[:, kb, :], at_, ki == 0 and h == 0, kb == 0 and h == 1,
                     reads=[vtB, atB_], writes=[psoB])
                if ki == 0:
                    P.add("dve", lambda e, sa_=sa_, sp_=sp_: e.tensor_copy(sa_, sp_), reads=[spB_], writes=[saB_])
                elif kb > 0:
                    P.add("dve", lambda e, sa_=sa_, sp_=sp_: e.tensor_tensor(sa_, sa_, sp_, ALU.add),
                          reads=[spB_, saB_], writes=[saB_])
                if kb == 0 and h == 1:
                    P.add("dve", lambda e, pso=pso, qcols=qcols: e.tensor_copy(od[:, qcols], pso), reads=[psoB], writes=[odB])

            stage1(units[0], 0)
            for ui in range(len(units)):
                if ui + 1 < len(units):
                    stage1(units[ui + 1], ui + 1)
                stage2(units[ui], ui)
            P.dma("sp", d["mixT"][s, 4 + hp], od, reads=[odB], writes=[ctx.Rf("mixT", s, 1)])


def phase_C(ctx, layer):
    P, A, d, nseq = ctx.P, ctx.A, ctx.d, ctx.nseq
    PS, PSB = ctx.PS, ctx.PSB
    even = layer % 2 == 0
    j = layer // 2
    P.barrier()
    A.reset()
    wout, woutB = A.alloc((8, D), BF16, "wout")
    P.dma("pool", wout, d["ev_w_out" if even else "od_w_out"][j].rearrange("(c p) n -> p c n", p=128), writes=[woutB])
    wr, wrB = A.alloc((8, 20), F32, "wr")
    P.dma("sp", wr, d["moe_wr"][layer].rearrange("(c p) n -> p c n", p=128), writes=[wrB])
    brb, brbB = A.alloc((4, 20), F32, "brb")
    for t in range(4):
        src = bass.AP(d["moe_br"].tensor, d["moe_br"][layer].offset, [[0, 128], [1, 20]])
        P.dma("sp", brb[:, t, :], src, writes=[brbB])
    scr = {k: A.alloc(4, F32, k) for k in ("ss", "lnv", "rstd")}
    scr["junk"] = A.alloc(1024, BF16, "junk")
    xb = [A.alloc((4, 1024), F32, "xt%d" % i) for i in range(2)]
    xn, xnB = A.alloc((4, 1024), F32, "xn")
    mxb = [A.alloc((8, 512), BF16, "mx%d" % i) for i in range(2)]
    hTb = [A.alloc((8, 512), BF16, "hT%d" % i) for i in range(2)]
    h32 = [A.alloc(512, F32, "h32_%d" % i) for i in range(2)]
    tmp = [A.alloc(512, F32, "tmp%d" % i) for i in range(2)]
    lg, lgB = A.alloc((4, 20), F32, "lg")
    small = {k: A.alloc(4, F32, "r_" + k) for k in ("gmax", "nmax", "gsum", "gw", "m1", "m2", "dm", "e21", "w1", "w2")}
    big = {k: A.alloc((4, 4), F32, "r_" + k) for k in ("ex", "oh", "el", "mask1", "el2", "mask2", "gsel")}
    scr2 = [scr, {k: A.alloc(4, F32, k + "b") for k in ("ss", "lnv", "rstd")}]
    scr2[1]["junk"] = scr["junk"]
    xnb = [(xn, xnB), A.alloc((4, 1024), F32, "xn1")]
    gsd = {}
    blocks = [(s, blk) for s in range(nseq) for blk in range(8)]
    src_x = d["x"] if layer == 0 else d["xs"]

    def s1(bi):
        s, blk = blocks[bi]
        if blk == 0:
            gsd[s] = make_gs_sh(ctx, layer, s, 3, 4, d["norm2_g"][layer], A, "2_%d" % s) + \
                load_bcast(ctx, d["mod"][layer, s, 2 * D:3 * D], D, A, "g1b%d" % s, reads=[ctx.Rf("mod")])
        gs, gsB, sh, shB, g1b, g1bB = gsd[s]
        xn, xnB = xnb[bi % 2]
        xt, xtB = xb[bi % 2]
        mx, mxB = mxb[bi % 2]
        cols = slice(blk * 512, (blk + 1) * 512)
        t0 = s * S + blk * 512
        cols = slice(blk * 512, (blk + 1) * 512)
        xt, xtB = xb[bi % 2]
        mx, mxB = mxb[bi % 2]
        hT, hTB = hTb[bi % 2]
        rd = [] if layer == 0 else [ctx.Rf("xs", s, blk)]
        P.dma("sp", xt, src_x[t0:t0 + 512, :].rearrange("(t p) dd -> p t dd", p=128), reads=rd, writes=[xtB])
        P.dma("sp", mx, d["mixT"][s].rearrange("h p t -> p h t")[:, :, cols],
              reads=[ctx.Rf("mixT", s, 0), ctx.Rf("mixT", s, 1)], writes=[mxB])
        k = 0
        for t in range(4):
            for half in range(2):
                ps, psB = PS[4 + k % 4], PSB[4 + k % 4]
                hs = slice(half * 512, (half + 1) * 512)
                for c in range(8):
                    P.mm(ps, mx[:, c, t * 128:(t + 1) * 128], wout[:, c, hs], c == 0, c == 7, reads=[mxB, woutB], writes=[psB])
                tm, tmB = tmp[k % 2]
                P.add("dve", lambda e, tm=tm, ps=ps, hs=hs, g1b=g1b: e.tensor_tensor(tm, ps, g1b[:, hs], ALU.mult),
                      reads=[psB, g1bB], writes=[tmB])
                P.add("pool", lambda e, tm=tm, xt=xt, t=t, hs=hs: e.tensor_tensor(xt[:, t, hs], xt[:, t, hs], tm, ALU.add),
                      reads=[tmB, xtB], writes=[xtB])
                k += 1
        P.dma("sp", d["xs"][t0:t0 + 512, :].rearrange("(t p) dd -> p t dd", p=128), xt, reads=[xtB], writes=[ctx.Rf("xs", s, blk)])
        P.add("pool", lambda e, xt=xt: e.tensor_copy(xn, xt), reads=[xtB], writes=[xnB])
        nmt_s1(ctx, xn, xnB, scr2[bi % 2])

    def s2(bi):
        s, blk = blocks[bi]
        gs, gsB, sh, shB, g1b, g1bB = gsd[s]
        xn, xnB = xnb[bi % 2]
        hT, hTB = hTb[bi % 2]
        psr, psrB = PS[2], PSB[2]
        psrv = psr[:, 0:128].rearrange("p (t n) -> p t n", t=4)

        def router(c, h, hB, psrv=psrv, psrB=psrB):
            for t in range(4):
                P.mm(psrv[:, t, 0:20], h[:, t * 128:(t + 1) * 128], wr[:, c, :], c == 0 and t == 0, c == 7,
                     reads=[hB, wrB], writes=[psrB])
        nmt_s2(ctx, xn, xnB, gs, gsB, sh, shB, hT, hTB, h32=h32, router=router)

    def s3(bi):
        s, blk = blocks[bi]
        t0 = s * S + blk * 512
        cols = slice(blk * 512, (blk + 1) * 512)
        hT, hTB = hTb[bi % 2]
        psr, psrB = PS[2], PSB[2]
        psrv = psr[:, 0:128].rearrange("p (t n) -> p t n", t=4)
        P.dma("sp", d["h2T"][s].rearrange("h p t -> p h t")[:, :, cols], hT, reads=[hTB], writes=[ctx.Rf("h2T", s, blk // 4)])
        V = lambda k_: small[k_][0]
        VB = lambda k_: small[k_][1]
        W = lambda k_: big[k_][0]
        WB = lambda k_: big[k_][1]
        dv = lambda fn, rd_, wr_: P.add("dve", fn, reads=rd_, writes=wr_)
        dv(lambda e, psrv=psrv: e.tensor_tensor(lg, psrv[:, :, 0:20], brb, ALU.add), [psrB, brbB], [lgB])
        dv(lambda e: e.tensor_reduce(V("gmax"), lg[:, :, 0:4], AX.X, ALU.max), [lgB], [VB("gmax")])
        dv(lambda e: e.tensor_scalar(V("nmax"), V("gmax"), -1.0, None, op0=ALU.mult), [VB("gmax")], [VB("nmax")])
        P.add("pool", lambda e: e.memset(V("gsum"), 0.0), writes=[VB("gsum")])
        for t in range(4):
            P.act(W("ex")[:, t, :], lg[:, t, 0:4], AF.Exp, reads=[lgB, VB("nmax")], writes=[WB("ex"), VB("gsum")],
                  bias=V("nmax")[:, t:t + 1], accum_out=V("gsum")[:, t:t + 1])
        dv(lambda e: e.reciprocal(V("gw"), V("gsum")), [VB("gsum")], [VB("gw")])
        for t in range(4):
            dv(lambda e, t=t: e.tensor_scalar(W("oh")[:, t, :], lg[:, t, 0:4], V("gmax")[:, t:t + 1], None, op0=ALU.is_equal),
               [lgB, VB("gmax")], [WB("oh")])
        for t in range(4):
            for g in range(4):
                src = lg[:, t, 4 + 4 * g:8 + 4 * g]
                if g == 0:
                    dv(lambda e, t=t, src=src: e.tensor_scalar(W("el")[:, t, :], src, W("oh")[:, t, 0:1], None, op0=ALU.mult),
                       [lgB, WB("oh")], [WB("el")])
                else:
                    dv(lambda e, t=t, g=g, src=src: e.scalar_tensor_tensor(W("el")[:, t, :], src, W("oh")[:, t, g:g + 1], W("el")[:, t, :], ALU.mult, ALU.add),
                       [lgB, WB("oh"), WB("el")], [WB("el")])
        dv(lambda e: e.tensor_reduce(V("m1"), W("el"), AX.X, ALU.max), [WB("el")], [VB("m1")])
        for t in range(4):
            dv(lambda e, t=t: e.tensor_scalar(W("mask1")[:, t, :], W("el")[:, t, :], V("m1")[:, t:t + 1], None, op0=ALU.is_equal),
               [WB("el"), VB("m1")], [WB("mask1")])
        dv(lambda e: e.scalar_tensor_tensor(W("el2"), W("mask1"), -1e30, W("el"), ALU.mult, ALU.add), [WB("mask1"), WB("el")], [WB("el2")])
        dv(lambda e: e.tensor_reduce(V("m2"), W("el2"), AX.X, ALU.max), [WB("el2")], [VB("m2")])
        for t in range(4):
            dv(lambda e, t=t: e.tensor_scalar(W("mask2")[:, t, :], W("el2")[:, t, :], V("m2")[:, t:t + 1], None, op0=ALU.is_equal),
               [WB("el2"), VB("m2")], [WB("mask2")])
        dv(lambda e: e.tensor_tensor(V("dm"), V("m2"), V("m1"), ALU.subtract), [VB("m1"), VB("m2")], [VB("dm")])
        P.act(V("e21"), V("dm"), AF.Exp, reads=[VB("dm")], writes=[VB("e21")])
        dv(lambda e: e.tensor_scalar(V("w1"), V("e21"), 1.0, None, op0=ALU.add), [VB("e21")], [VB("w1")])
        dv(lambda e: e.reciprocal(V("w1"), V("w1")), [VB("w1")], [VB("w1")])
        dv(lambda e: e.tensor_tensor(V("w2"), V("e21"), V("w1"), ALU.mult), [VB("e21"), VB("w1")], [VB("w2")])
        dv(lambda e: e.tensor_tensor(V("w1"), V("w1"), V("gw"), ALU.mult), [VB("w1"), VB("gw")], [VB("w1")])
        dv(lambda e: e.tensor_tensor(V("w2"), V("w2"), V("gw"), ALU.mult), [VB("w2"), VB("gw")], [VB("w2")])
        for t in range(4):
            dv(lambda e, t=t: e.tensor_scalar(W("gsel")[:, t, :], W("mask1")[:, t, :], V("w1")[:, t:t + 1], None, op0=ALU.mult),
               [WB("mask1"), VB("w1")], [WB("gsel")])
            dv(lambda e, t=t: e.scalar_tensor_tensor(W("gsel")[:, t, :], W("mask2")[:, t, :], V("w2")[:, t:t + 1], W("gsel")[:, t, :], ALU.mult, ALU.add),
               [WB("mask2"), VB("w2"), WB("gsel")], [WB("gsel")])
        for t in range(4):
            tile_g = s * 32 + blk * 4 + t
            for g in range(4):
                dv(lambda e, t=t, g=g, tile_g=tile_g: e.tensor_scalar(
                    ctx.gates[:, tile_g, 4 * g:4 * g + 4], W("gsel")[:, t, :], W("oh")[:, t, g:g + 1], None, op0=ALU.mult),
                   [WB("gsel"), WB("oh")], [ctx.gatesB])
        if ctx.debug:
            P.dma("sp", d["gates_dbg"][t0:t0 + 512, :].rearrange("(t p) n -> p t n", p=128),
                  ctx.gates[:, s * 32 + blk * 4:s * 32 + blk * 4 + 4, :], reads=[ctx.gatesB])


    pipeline3(len(blocks), s1, s2, s3)


def phase_D(ctx, layer):
    P, A, d, nseq = ctx.P, ctx.A, ctx.d, ctx.nseq
    PS, PSB = ctx.PS, ctx.PSB
    last = layer == DEPTH - 1
    P.barrier()
    A.reset()
    SB = 2048
    nsb = nseq * S // SB
    hT, hTB = A.alloc((8, SB), BF16, "hT")
    yacc, yaccB = A.alloc((16, D), F32, "yacc")
    wts = []
    for i in range(2):
        wts.append((A.alloc((8, 512), BF16, "wg%d" % i), A.alloc((8, 512), BF16, "wu%d" % i), A.alloc((4, D), BF16, "wd%d" % i)))
    hid = [A.alloc((4, 512), BF16, "hid%d" % i) for i in range(2)]
    sg = [A.alloc(512, F32, "sg%d" % i) for i in range(2)]
    xtb = [A.alloc(D, F32, "xt%d" % i) for i in range(2)]
    ss, ssB = A.alloc(1, F32, "fss")
    junk, junkB = A.alloc(D, BF16, "fjunk")
    if last:
        gfb, gfbB = load_bcast(ctx, d["final_norm_g"], D, A, "gfb")
    ei = 0
    for sb in range(nsb):
        s = sb // 2
        half_s = sb % 2
        g2b, g2bB = load_bcast(ctx, d["mod"][layer, s, 5 * D:6 * D], D, A, "g2b%d" % sb, reads=[ctx.Rf("mod")])
        P.dma("sp", hT, d["h2T"][s].rearrange("h p t -> p h t")[:, :, half_s * SB:(half_s + 1) * SB],
              reads=[ctx.Rf("h2T", s, half_s)], writes=[hTB])
        wsel = {}

        def load_w(ex):
            nonlocal ei
            w3 = wts[ei % 2]
            ei += 1
            (wg, wgB), (wu, wuB), (wd, wdB) = w3
            P.dma("pool", wg, d["moe_w_gate"][layer, ex].rearrange("(c p) n -> p c n", p=128), writes=[wgB])
            P.dma("pool", wu, d["moe_w_up"][layer, ex].rearrange("(c p) n -> p c n", p=128), writes=[wuB])
            P.dma("pool", wd, d["moe_w_down"][layer, ex].rearrange("(c p) n -> p c n", p=128), writes=[wdB])
            wsel[ex] = w3

        def stage1(ex, blk, si):
            if blk == 0:
                load_w(ex)
            (wg, wgB), (wu, wuB), (wd, wdB) = wsel[ex]
            cols = slice(blk * 512, (blk + 1) * 512)
            hd, hdB = hid[si % 2]
            for fc in range(4):
                psg, psgB = PS[fc % 2], PSB[fc % 2]
                psu, psuB = PS[2 + fc % 2], PSB[2 + fc % 2]
                for c in range(8):
                    P.mm(psg, wg[:, c, fc * 128:(fc + 1) * 128], hT[:, c, cols], c == 0, c == 7, reads=[wgB, hTB], writes=[psgB])
                for c in range(8):
                    P.mm(psu, wu[:, c, fc * 128:(fc + 1) * 128], hT[:, c, cols], c == 0, c == 7, reads=[wuB, hTB], writes=[psuB])
                sg_, sgB_ = sg[fc % 2]
                P.act(sg_, psg, AF.Silu, reads=[psgB], writes=[sgB_])
                P.add("dve", lambda e, hd=hd, fc=fc, sg_=sg_, psu=psu: e.tensor_tensor(hd[:, fc, :], sg_, psu, ALU.mult),
                      reads=[sgB_, psuB], writes=[hdB])

        def stage2(ex, blk, si):
            (wg, wgB), (wu, wuB), (wd, wdB) = wsel[ex]
            hd, hdB = hid[si % 2]
            k = 0
            for t in range(4):
                tl = blk * 4 + t
                tile_g = sb * 16 + tl
                for half in range(2):
                    hs = slice(half * 512, (half + 1) * 512)
                    psy, psyB = PS[4 + k % 4], PSB[4 + k % 4]
                    k += 1
                    for fc in range(4):
                        P.mm(psy, hd[:, fc, t * 128:(t + 1) * 128], wd[:, fc, hs], fc == 0, fc == 3, reads=[hdB, wdB], writes=[psyB])
                    if ex == 0:
                        P.add("dve", lambda e, tl=tl, hs=hs, psy=psy, tile_g=tile_g, ex=ex: e.tensor_scalar(
                            yacc[:, tl, hs], psy, ctx.gates[:, tile_g, ex:ex + 1], None, op0=ALU.mult),
                            reads=[psyB, ctx.gatesB], writes=[yaccB])
                    else:
                        P.add("dve", lambda e, tl=tl, hs=hs, psy=psy, tile_g=tile_g, ex=ex: e.scalar_tensor_tensor(
                            yacc[:, tl, hs], psy, ctx.gates[:, tile_g, ex:ex + 1], yacc[:, tl, hs], ALU.mult, ALU.add),
                            reads=[psyB, ctx.gatesB, yaccB], writes=[yaccB])

        steps = [(ex, blk) for ex in range(16) for blk in range(4)]
        stage1(steps[0][0], steps[0][1], 0)
        for si in range(len(steps)):
            if si + 1 < len(steps):
                stage1(steps[si + 1][0], steps[si + 1][1], si + 1)
            stage2(steps[si][0], steps[si][1], si)
        if ctx.debug and layer == 0:
            P.dma("sp", d["yacc_dbg"][sb * SB:(sb + 1) * SB, :].rearrange("(t p) n -> p t n", p=128), yacc, reads=[yaccB])
        for tl in range(16):
            t0 = sb * SB + tl * 128
            blk_g = (t0 % S) // 512
            xt, xtB = xtb[tl % 2]
            P.dma("sp", xt, d["xs"][t0:t0 + 128, :], reads=[ctx.Rf("xs", s, blk_g)], writes=[xtB])
            P.add("pool", lambda e, tl=tl, g2b=g2b: e.tensor_tensor(yacc[:, tl, :], yacc[:, tl, :], g2b, ALU.mult), reads=[yaccB, g2bB], writes=[yaccB])
            P.add("pool", lambda e, tl=tl, xt=xt: e.tensor_tensor(xt, xt, yacc[:, tl, :], ALU.add), reads=[yaccB, xtB], writes=[xtB])
            if not last:
                P.dma("sp", d["xs"][t0:t0 + 128, :], xt, reads=[xtB], writes=[ctx.Rf("xs", s, blk_g)])
            else:
                P.add("pool", lambda e: e.memset(ss, 0.0), writes=[ssB])
                P.act(junk, xt, AF.Square, reads=[xtB], writes=[junkB, ssB], accum_out=ss)
                P.act(ss, ss, AF.Ln, reads=[ssB], writes=[ssB], scale=1.0 / D, bias=EPS)
                P.act(ss, ss, AF.Exp, reads=[ssB], writes=[ssB], scale=-0.5)
                P.add("dve", lambda e, xt=xt: e.tensor_scalar(xt, xt, ss[:, 0:1], None, op0=ALU.mult), reads=[xtB, ssB], writes=[xtB])
                P.add("dve", lambda e, xt=xt: e.tensor_tensor(xt, xt, gfb, ALU.mult), reads=[xtB, gfbB], writes=[xtB])
                P.dma("sp", d["out"][t0:t0 + 128, :], xt, reads=[xtB])


def prep_shared(inp):
    f = lambda a: np.ascontiguousarray(np.asarray(a, dtype=np.float32))
    sh = {}
    sh["biasT"] = _bias_tables(f(inp["rel_bias"]))
    for k in ("norm1_g", "norm2_g", "w_ada", "b_ada", "ev_w_in", "ev_w_out", "ev_gmlp_ln_g", "ev_gmlp_ln_b",
              "ev_b_s", "od_w_in", "od_w_out", "od_w_dw", "od_b_dw", "od_conv_ln_g", "od_conv_ln_b",
              "moe_w_gate", "moe_w_up", "moe_w_down", "final_norm_g"):
        sh[k] = f(inp[k])
    sh["ev_w_sT"] = f(np.transpose(np.asarray(inp["ev_w_s"]), (0, 1, 3, 2)))
    wg = np.asarray(inp["moe_w_group"])
    wrt = np.asarray(inp["moe_w_router"])
    sh["moe_wr"] = f(np.concatenate([wg, np.transpose(wrt, (0, 2, 1, 3)).reshape(DEPTH, D, 16)], axis=2))
    sh["moe_br"] = f(np.concatenate([np.asarray(inp["moe_b_group"]), np.asarray(inp["moe_b_router"]).reshape(DEPTH, 16)], axis=1))
    sh.update(_consts())
    return sh


def core_inputs(inp, shared, seqs):
    m = dict(shared)
    m["x"] = np.ascontiguousarray(np.asarray(inp["x"], dtype=np.float32)[seqs].reshape(len(seqs) * S, D))
    m["c"] = np.ascontiguousarray(np.asarray(inp["c"], dtype=np.float32)[seqs])
    return m


_CACHE = {}


def kernel(**inputs):
    nseq = 2
    if "nc" not in _CACHE:
        _CACHE["nc"] = build(nseq)[0]
    nc = _CACHE["nc"]
    shared = prep_shared(inputs)
    in_maps = [core_inputs(inputs, shared, list(range(i * nseq, (i + 1) * nseq))) for i in range(NCORES)]
    res = run_bass_kernel_spmd(nc, in_maps, core_ids=list(range(NCORES)))
    out = np.concatenate([np.asarray(r["out"]).reshape(nseq, S, D) for r in res.results], axis=0)
    return out.astype(np.float32)
```
